# Optimizing a Trainium2 kernel written in Bass

```python
import jax
import jax.numpy as jnp
from jax import lax
import numpy as np

D_MODEL = 2048
BATCH = 1
SEQ = 8192
DEPTH = 2

GRID_W = 64
CTX_LEN = 256
NORM_EPS = 1e-6
N_MOD = 6
POS_BASE = 10000.0

BR = 512
N_BRANCH = 4
FN_GROUPS = 4
FN_GC = BR // FN_GROUPS
RW_HEADS = 8
RW_HD = BR // RW_HEADS
RW_LORA_W = 32
RW_LORA_A = 32
RW_LORA_G = 64
RW_GN_EPS = 64e-5
HG_HEADS = 4
HG_DK = BR // HG_HEADS
HG_DV = BR // HG_HEADS
HG_CHUNK = 16
LRU_BLOCKS = 8
LRU_BD = BR // LRU_BLOCKS
CONV_W = 4
CONV_LEFT = 2
LRU_C = 8.0

COL_A = 0
COL_B = 1
COL_C = 5
COL_D = 10
N_COL_BLOCKS = 12
N_MIX_COLS = N_COL_BLOCKS * BR
N_IN = N_MIX_COLS + N_BRANCH * D_MODEL

N_EXPERTS = 64
N_GROUPS = 8
EXPERTS_PER_GROUP = N_EXPERTS // N_GROUPS
TOP_K = 2
D_EXPERT = 768
MOE_BLOCK = 128

kernel_name = "hybrid_fourier_rwkv7_hgrn2_rglru_moe_dit"


def rms_norm(x, g, eps=NORM_EPS):
    xf = x.astype(jnp.float32)
    y = xf * lax.rsqrt(jnp.mean(xf * xf, axis=-1, keepdims=True) + eps)
    return (y * g.astype(jnp.float32)).astype(x.dtype)


def split_heads(t, n_heads):
    return t.reshape(t.shape[:-1] + (n_heads, t.shape[-1] // n_heads))


def merge_heads(t):
    return t.reshape(t.shape[:-2] + (t.shape[-2] * t.shape[-1],))


def time_shift(u, off):
    L = u.shape[1]
    p = abs(off)
    widths = [(0, 0)] * u.ndim
    widths[1] = (p, p)
    return lax.slice_in_dim(jnp.pad(u, widths), p + off, p + off + L, axis=1)


def bidir_token_shift(u):
    return 0.5 * (time_shift(u, -1) + time_shift(u, 1)) - u


def depthwise_conv(u, w, b):
    out = b
    for j in range(CONV_W):
        out = out + time_shift(u, j - CONV_LEFT) * w[j]
    return out


def prefix_scan(scan_fn, ctx_in, lat_in, s0, reverse):
    if reverse:
        ctx_in = tuple(jnp.flip(t, axis=1) for t in ctx_in)
        lat_in = tuple(jnp.flip(t, axis=1) for t in lat_in)
    y_ctx, s_ctx = scan_fn(*ctx_in, s0)
    y_lat, _ = scan_fn(*lat_in, s_ctx)
    if reverse:
        y_ctx, y_lat = jnp.flip(y_ctx, axis=1), jnp.flip(y_lat, axis=1)
    return y_ctx, y_lat


def grid_sincos(rows):
    t = jnp.arange(rows * GRID_W)
    r = (t // GRID_W).astype(jnp.float32)
    col = (t % GRID_W).astype(jnp.float32)
    nf = D_MODEL // 4
    omega = POS_BASE ** (-jnp.arange(nf, dtype=jnp.float32) / nf)
    ang_r = r[:, None] * omega
    ang_c = col[:, None] * omega
    return jnp.concatenate([jnp.sin(ang_r), jnp.cos(ang_r), jnp.sin(ang_c), jnp.cos(ang_c)], axis=-1)


def fourier_group(u):
    B_, L, _ = u.shape
    ug = u.astype(jnp.float32).reshape(B_, L, FN_GROUPS, FN_GC)
    return jnp.fft.fft2(ug, axes=(1, 3), norm="ortho").real.reshape(B_, L, BR)


def rwkv7_scan(r, wl, k, v, kk, a, s0):
    def step(S, inp):
        r_t, wl_t, k_t, v_t, kk_t, a_t = inp
        S = (S * jnp.exp(wl_t)[:, :, None, :]
             - jnp.einsum("bhvk,bhk->bhv", S, kk_t)[..., None] * (a_t * kk_t)[:, :, None, :]
             + v_t[..., :, None] * k_t[:, :, None, :])
        return S, jnp.einsum("bhvk,bhk->bhv", S, r_t)
    xs = tuple(jnp.moveaxis(t, 1, 0) for t in (r, wl, k, v, kk, a))
    s_fin, ys = lax.scan(step, s0, xs)
    return jnp.moveaxis(ys, 0, 1), s_fin


def rwkv7_mixer(uc, ul, mu, w0, w1, w2, a0, a1, a2, g1, g2, k_k, k_a, r_k, ln_g, ln_b):
    def prep(u):
        u = u.astype(jnp.float32)
        xx = bidir_token_shift(u)
        r = u[:, :, 0] + xx[:, :, 0] * mu[0]
        k = u[:, :, 1] + xx[:, :, 1] * mu[1]
        v = u[:, :, 2] + xx[:, :, 2] * mu[2]
        xw = u[:, :, 3] + xx[:, :, 3] * mu[3]
        xa = u[:, :, 3] + xx[:, :, 3] * mu[4]
        xg = u[:, :, 3] + xx[:, :, 3] * mu[5]
        kk = split_heads(k * k_k, RW_HEADS)
        kk = kk / jnp.maximum(jnp.sqrt(jnp.sum(kk * kk, axis=-1, keepdims=True)), 1e-12)
        p = {"r": split_heads(r, RW_HEADS), "v": split_heads(v, RW_HEADS), "kk": kk,
             "g": jax.nn.sigmoid(xg @ g1) @ g2, "wl": [], "kt": [], "a": []}
        for d in range(2):
            w_raw = w0[d] + jnp.tanh(xw @ w1[d]) @ w2[d]
            a = jax.nn.sigmoid(a0[d] + (xa @ a1[d]) @ a2[d])
            p["wl"].append(split_heads(-jnp.exp(-jax.nn.softplus(-w_raw) - 0.5), RW_HEADS))
            p["kt"].append(split_heads(k * (1.0 + (a - 1.0) * k_a), RW_HEADS))
            p["a"].append(split_heads(a, RW_HEADS))
        return p

    def scan_inputs(p, d):
        return (p["r"], p["wl"][d], p["kt"][d], p["v"], p["kk"], p["a"][d])

    def post(y, p):
        mean = jnp.mean(y, axis=-1, keepdims=True)
        var = jnp.mean(jnp.square(y - mean), axis=-1, keepdims=True)
        y = (y - mean) * lax.rsqrt(var + RW_GN_EPS) * split_heads(ln_g, RW_HEADS) + split_heads(ln_b, RW_HEADS)
        bonus = jnp.sum(p["r"] * (p["kt"][0] + p["kt"][1]) * r_k, axis=-1, keepdims=True) * p["v"]
        return merge_heads(y + bonus) * p["g"]

    pc, pl = prep(uc), prep(ul)
    s0 = jnp.zeros((uc.shape[0], RW_HEADS, RW_HD, RW_HD), jnp.float32)
    y_c, y_l = 0.0, 0.0
    for d in range(2):
        yc_d, yl_d = prefix_scan(rwkv7_scan, scan_inputs(pc, d), scan_inputs(pl, d), s0, reverse=(d == 1))
        y_c, y_l = y_c + yc_d, y_l + yl_d
    return jnp.concatenate([post(y_c, pc), post(y_l, pl)], axis=1)


def hgrn2_chunk_scan(q, lf, k, v, s0):
    B_, L, H, _ = q.shape
    DV = v.shape[-1]
    n = L // HG_CHUNK

    def blocks(t):
        return t.reshape(B_, n, HG_CHUNK, H, t.shape[-1]).transpose(0, 3, 1, 2, 4)

    q, lf, k, v = blocks(q), blocks(lf), blocks(k), blocks(v)
    G = jnp.cumsum(lf, axis=3)
    lower = jnp.tril(jnp.ones((HG_CHUNK, HG_CHUNK), bool))[:, :, None]
    diff = G[:, :, :, :, None, :] - G[:, :, :, None, :, :]
    decay = jnp.exp(jnp.where(lower, diff, -jnp.inf))
    scores = jnp.einsum("bhntc,bhnsc,bhntsc->bhnts", q, k, decay)
    o_intra = jnp.einsum("bhnts,bhnsv->bhntv", scores, v)
    G_last = G[:, :, :, -1:, :]
    U = jnp.einsum("bhncd,bhncv->bhndv", k * jnp.exp(G_last - G), v)
    chunk_decay = jnp.exp(G_last[:, :, :, 0])

    def step(S, inp):
        dec, u = inp
        return dec[..., None] * S + u, S

    s_fin, S_start = lax.scan(step, s0, (jnp.moveaxis(chunk_decay, 2, 0), jnp.moveaxis(U, 2, 0)))
    S_start = jnp.moveaxis(S_start, 0, 2)
    o_inter = jnp.einsum("bhncd,bhndv->bhncv", q * jnp.exp(G), S_start)
    o = (o_intra + o_inter).transpose(0, 2, 3, 1, 4).reshape(B_, L, H, DV)
    return o, s_fin


def hgrn2_mixer(uc, ul, lb, norm_g):
    lb = lb.astype(jnp.float32)
    log_lb, log_1mlb = jnp.log(lb), jnp.log1p(-lb)

    def prep(u):
        u = u.astype(jnp.float32)
        p = {"q": split_heads(jax.nn.silu(u[:, :, 0]), HG_HEADS), "i": split_heads(u[:, :, 3], HG_HEADS),
             "og": u[:, :, 4], "lf": [], "k": []}
        for d in range(2):
            z = u[:, :, 1 + d]
            p["lf"].append(split_heads(jnp.logaddexp(log_lb, log_1mlb + jax.nn.log_sigmoid(z)), HG_HEADS))
            p["k"].append(split_heads((1.0 - lb) * jax.nn.sigmoid(-z), HG_HEADS))
        return p

    def post(y, p):
        y = y * lax.rsqrt(jnp.mean(y * y, axis=-1, keepdims=True) + NORM_EPS) * norm_g
        return merge_heads(y) * jax.nn.silu(p["og"])

    pc, pl = prep(uc), prep(ul)
    s0 = jnp.zeros((uc.shape[0], HG_HEADS, HG_DK, HG_DV), jnp.float32)
    y_c, y_l = 0.0, 0.0
    for d in range(2):
        ins_c = (pc["q"], pc["lf"][d], pc["k"][d], pc["i"])
        ins_l = (pl["q"], pl["lf"][d], pl["k"][d], pl["i"])
        yc_d, yl_d = prefix_scan(hgrn2_chunk_scan, ins_c, ins_l, s0, reverse=(d == 1))
        y_c, y_l = y_c + yc_d, y_l + yl_d
    return jnp.concatenate([post(y_c, pc), post(y_l, pl)], axis=1)


def rglru_scan(a, b, h0):
    def combine(left, right):
        a_l, b_l = left
        a_r, b_r = right
        return a_l * a_r, a_r * b_l + b_r
    a_cum, b_cum = lax.associative_scan(combine, (a, b), axis=1)
    h = a_cum * h0[:, None] + b_cum
    return h, h[:, -1]


def rglru_mixer(uc, ul, conv_w, conv_b, wa, ba, wx, bx, lam):
    def prep(u):
        u = u.astype(jnp.float32)
        xc = depthwise_conv(u[:, :, 0], conv_w, conv_b)
        xb = split_heads(xc, LRU_BLOCKS)
        ins = []
        for d in range(2):
            r = jax.nn.sigmoid(merge_heads(jnp.einsum("blgi,gij->blgj", xb, wa[d])) + ba[d])
            i = jax.nn.sigmoid(merge_heads(jnp.einsum("blgi,gij->blgj", xb, wx[d])) + bx[d])
            log_a = -LRU_C * r * jax.nn.softplus(-lam[d])
            mult = jnp.sqrt(-jnp.expm1(2.0 * log_a))
            ins.append((jnp.exp(log_a), mult * i * xc))
        return ins, jax.nn.gelu(u[:, :, 1])

    (ins_c, gate_c), (ins_l, gate_l) = prep(uc), prep(ul)
    s0 = jnp.zeros((uc.shape[0], BR), jnp.float32)
    y_c, y_l = 0.0, 0.0
    for d in range(2):
        yc_d, yl_d = prefix_scan(rglru_scan, ins_c[d], ins_l[d], s0, reverse=(d == 1))
        y_c, y_l = y_c + yc_d, y_l + yl_d
    return jnp.concatenate([y_c * gate_c, y_l * gate_l], axis=1)


def token_mixers(h_ctx, h_lat, w_in, w_branch, w_out,
                 rw_mu, rw_w0, rw_w1, rw_w2, rw_a0, rw_a1, rw_a2, rw_g1, rw_g2, rw_kk, rw_ka, rw_rk,
                 rw_ln_g, rw_ln_b, hg_lb, hg_norm_g,
                 lru_conv_w, lru_conv_b, lru_wa, lru_ba, lru_wx, lru_bx, lru_lam):
    B_, L, D = h_ctx.shape
    S = L + h_lat.shape[1]
    h_all = jnp.concatenate([h_ctx, h_lat], axis=1)
    proj = h_all @ w_in
    cols = proj[..., :N_MIX_COLS].reshape(B_, S, N_COL_BLOCKS, BR)
    gates = jax.nn.sigmoid(proj[..., N_MIX_COLS:].astype(jnp.float32)).reshape(B_, S, N_BRANCH, D)
    cc, cl = cols[:, :L], cols[:, L:]
    y_a = jnp.concatenate([fourier_group(cc[:, :, COL_A]), fourier_group(cl[:, :, COL_A])], axis=1)
    y_b = rwkv7_mixer(cc[:, :, COL_B:COL_B + 4], cl[:, :, COL_B:COL_B + 4], rw_mu, rw_w0, rw_w1, rw_w2,
                      rw_a0, rw_a1, rw_a2, rw_g1, rw_g2, rw_kk, rw_ka, rw_rk, rw_ln_g, rw_ln_b)
    y_c = hgrn2_mixer(cc[:, :, COL_C:COL_C + 5], cl[:, :, COL_C:COL_C + 5], hg_lb, hg_norm_g)
    y_d = rglru_mixer(cc[:, :, COL_D:COL_D + 2], cl[:, :, COL_D:COL_D + 2], lru_conv_w, lru_conv_b,
                      lru_wa, lru_ba, lru_wx, lru_bx, lru_lam)
    branches = jnp.stack([y_a, y_b, y_c, y_d], axis=2).astype(h_all.dtype)
    proj_br = jnp.einsum("bskc,kcd->bskd", branches, w_branch)
    merged = jnp.sum(gates.astype(h_all.dtype) * proj_br, axis=2)
    out = merged @ w_out
    return out[:, :L], out[:, L:]


def moe_ffn(h, router_w, router_b, w_gate, w_up, w_down):
    N, D = h.shape
    scores = jax.nn.sigmoid(jnp.dot(h.astype(jnp.float32), router_w.astype(jnp.float32)))
    sel = (scores + router_b.astype(jnp.float32)).reshape(N, N_GROUPS, EXPERTS_PER_GROUP)
    group_score = jnp.sum(lax.top_k(sel, TOP_K)[0], axis=-1)
    g_idx = jnp.argmax(group_score, axis=-1)
    in_grp = jnp.take_along_axis(sel, g_idx[:, None, None], axis=1)[:, 0]
    _, e_local = lax.top_k(in_grp, TOP_K)
    e_idx = g_idx[:, None] * EXPERTS_PER_GROUP + e_local
    wts = jnp.take_along_axis(scores, e_idx, axis=1)
    wts = wts / jnp.sum(wts, axis=-1, keepdims=True)
    n_assign = N * TOP_K
    flat_e = e_idx.reshape(-1)
    order = jnp.argsort(flat_e)
    sorted_e = flat_e[order]
    tok = order // TOP_K
    sizes = jnp.bincount(flat_e, length=N_EXPERTS)
    starts = jnp.cumsum(sizes) - sizes
    padded = ((sizes + MOE_BLOCK - 1) // MOE_BLOCK) * MOE_BLOCK
    pad_ends = jnp.cumsum(padded)
    pad_starts = pad_ends - padded
    dest = pad_starts[sorted_e] + jnp.arange(n_assign) - starts[sorted_e]
    n_blocks = -(-n_assign // MOE_BLOCK) + N_EXPERTS
    buf = jnp.zeros((n_blocks * MOE_BLOCK, D), h.dtype).at[dest].set(h[tok])
    blk_e = jnp.minimum(jnp.searchsorted(pad_ends, jnp.arange(n_blocks) * MOE_BLOCK, side="right"),
                        N_EXPERTS - 1)

    def expert_block(args):
        xb, e = args
        return (jax.nn.silu(xb @ w_gate[e]) * (xb @ w_up[e])) @ w_down[e]

    yb = lax.map(expert_block, (buf.reshape(n_blocks, MOE_BLOCK, D), blk_e))
    y = yb.reshape(-1, D)[dest] * wts.reshape(-1)[order][:, None].astype(h.dtype)
    return jnp.zeros_like(h).at[tok].add(y)


def setup_inputs(seed: int = 0) -> dict:
    key = jax.random.key(seed)
    keys = iter(jax.random.split(key, 48))

    def nrm(shape, scale):
        return jax.random.normal(next(keys), shape, jnp.float32) * scale

    def unif(shape, lo, hi):
        return jax.random.uniform(next(keys), shape, jnp.float32, lo, hi)

    D = D_MODEL
    lru_a = unif((DEPTH, 2, BR), 0.9, 0.999) ** (1.0 / LRU_C)
    return {
        "x": nrm((BATCH, SEQ, D), 1.0),
        "c": nrm((BATCH, D), 1.0),
        "ctx": nrm((BATCH, CTX_LEN, D), 1.0),
        "c_ctx": nrm((D,), 1.0),
        "w_mod": nrm((DEPTH, D, N_MOD * D), 0.5 * D ** -0.5),
        "b_mod": nrm((DEPTH, N_MOD * D), 0.02),
        "norm_mix_g": 1.0 + nrm((DEPTH, D), 0.02),
        "norm_ffn_g": 1.0 + nrm((DEPTH, D), 0.02),
        "w_in": nrm((DEPTH, D, N_IN), D ** -0.5),
        "w_branch": nrm((DEPTH, N_BRANCH, BR, D), BR ** -0.5),
        "w_out": nrm((DEPTH, D, D), D ** -0.5),
        "rw_mu": unif((DEPTH, 6, BR), 0.0, 1.0),
        "rw_w0": unif((DEPTH, 2, BR), -6.0, -1.0),
        "rw_w1": nrm((DEPTH, 2, BR, RW_LORA_W), BR ** -0.5),
        "rw_w2": nrm((DEPTH, 2, RW_LORA_W, BR), 0.1 * RW_LORA_W ** -0.5),
        "rw_a0": nrm((DEPTH, 2, BR), 0.1),
        "rw_a1": nrm((DEPTH, 2, BR, RW_LORA_A), BR ** -0.5),
        "rw_a2": nrm((DEPTH, 2, RW_LORA_A, BR), RW_LORA_A ** -0.5),
        "rw_g1": nrm((DEPTH, BR, RW_LORA_G), BR ** -0.5),
        "rw_g2": nrm((DEPTH, RW_LORA_G, BR), RW_LORA_G ** -0.5),
        "rw_kk": 0.85 + nrm((DEPTH, BR), 0.05),
        "rw_ka": 1.0 + nrm((DEPTH, BR), 0.05),
        "rw_rk": nrm((DEPTH, RW_HEADS, RW_HD), 0.1),
        "rw_ln_g": 1.0 + nrm((DEPTH, BR), 0.02),
        "rw_ln_b": nrm((DEPTH, BR), 0.02),
        "hg_lb_logits": nrm((DEPTH, BR), 0.1),
        "hg_norm_g": 1.0 + nrm((DEPTH, HG_DV), 0.02),
        "lru_conv_w": nrm((DEPTH, CONV_W, BR), CONV_W ** -0.5),
        "lru_conv_b": nrm((DEPTH, BR), 0.02),
        "lru_wa": nrm((DEPTH, 2, LRU_BLOCKS, LRU_BD, LRU_BD), LRU_BD ** -0.5),
        "lru_ba": nrm((DEPTH, 2, BR), 0.02),
        "lru_wx": nrm((DEPTH, 2, LRU_BLOCKS, LRU_BD, LRU_BD), LRU_BD ** -0.5),
        "lru_bx": nrm((DEPTH, 2, BR), 0.02),
        "lru_lam": jnp.log(lru_a) - jnp.log1p(-lru_a),
        "router_w": nrm((D, N_EXPERTS), D ** -0.5),
        "router_b": nrm((N_EXPERTS,), 0.01),
        "moe_w_gate": nrm((DEPTH, N_EXPERTS, D, D_EXPERT), D ** -0.5),
        "moe_w_up": nrm((DEPTH, N_EXPERTS, D, D_EXPERT), D ** -0.5),
        "moe_w_down": nrm((DEPTH, N_EXPERTS, D_EXPERT, D), D_EXPERT ** -0.5),
        "final_norm_g": 1.0 + nrm((D,), 0.02),
    }


def reference(x, c, ctx, c_ctx, w_mod, b_mod, norm_mix_g, norm_ffn_g, w_in, w_branch, w_out,
              rw_mu, rw_w0, rw_w1, rw_w2, rw_a0, rw_a1, rw_a2, rw_g1, rw_g2, rw_kk, rw_ka, rw_rk,
              rw_ln_g, rw_ln_b, hg_lb_logits, hg_norm_g, lru_conv_w, lru_conv_b, lru_wa, lru_ba,
              lru_wx, lru_bx, lru_lam, router_w, router_b, moe_w_gate, moe_w_up, moe_w_down,
              final_norm_g):
    B_, T, D = x.shape
    L = ctx.shape[1]
    ROWS = T // GRID_W
    x = x + grid_sincos(ROWS).astype(x.dtype)[None]
    lb_cum = jnp.cumsum(jax.nn.softmax(hg_lb_logits.astype(jnp.float32), axis=0), axis=0)
    hg_lb = lb_cum - lb_cum[:1]
    sc, scc = jax.nn.silu(c), jax.nn.silu(c_ctx)
    for l in range(DEPTH):
        last = l == DEPTH - 1
        m_lat = (sc @ w_mod[l] + b_mod[l]).reshape(B_, N_MOD, 1, D)
        m_ctx = (scc @ w_mod[l] + b_mod[l]).reshape(N_MOD, D)
        h_lat = rms_norm(x, norm_mix_g[l]) * (1.0 + m_lat[:, 1]) + m_lat[:, 0]
        h_ctx = rms_norm(ctx, norm_mix_g[l]) * (1.0 + m_ctx[1]) + m_ctx[0]
        mix_ctx, mix_lat = token_mixers(
            h_ctx, h_lat, w_in[l], w_branch[l], w_out[l],
            rw_mu[l], rw_w0[l], rw_w1[l], rw_w2[l], rw_a0[l], rw_a1[l], rw_a2[l], rw_g1[l], rw_g2[l],
            rw_kk[l], rw_ka[l], rw_rk[l], rw_ln_g[l], rw_ln_b[l], hg_lb[l], hg_norm_g[l],
            lru_conv_w[l], lru_conv_b[l], lru_wa[l], lru_ba[l], lru_wx[l], lru_bx[l], lru_lam[l])
        x = x + m_lat[:, 2] * mix_lat
        h_lat = rms_norm(x, norm_ffn_g[l]) * (1.0 + m_lat[:, 4]) + m_lat[:, 3]
        if last:
            y_lat = moe_ffn(h_lat.reshape(-1, D), router_w, router_b,
                            moe_w_gate[l], moe_w_up[l], moe_w_down[l]).reshape(B_, T, D)
            x = x + m_lat[:, 5] * y_lat
        else:
            ctx = ctx + m_ctx[2] * mix_ctx
            h_ctx = rms_norm(ctx, norm_ffn_g[l]) * (1.0 + m_ctx[4]) + m_ctx[3]
            h_all = jnp.concatenate([h_ctx, h_lat], axis=1).reshape(-1, D)
            y = moe_ffn(h_all, router_w, router_b,
                        moe_w_gate[l], moe_w_up[l], moe_w_down[l]).reshape(B_, L + T, D)
            ctx = ctx + m_ctx[5] * y[:, :L]
            x = x + m_lat[:, 5] * y[:, L:]
    return rms_norm(x, final_norm_g)
```

```python
import numpy as np
import concourse.bass as bass
import concourse.mybir as mybir
from concourse.bass_utils import run_bass_kernel_spmd
from contextlib import ExitStack

F32 = mybir.dt.float32
BF16 = mybir.dt.bfloat16
I32 = mybir.dt.int32
U32 = mybir.dt.uint32
AF = mybir.ActivationFunctionType
ALU = mybir.AluOpType
AX = mybir.AxisListType

SEM_GEN = 20000
N_DMA_SEMS = 12


class KB:
    ENG = ("pe", "dve", "act", "pool", "sp")

    def __init__(self, nc):
        self.nc = nc
        self.es = ExitStack()
        self.q = {e: [] for e in self.ENG}
        self.lastw = {}
        self.readers = {}
        self.n_t = 0
        self.dma_count = {e: 0 for e in self.ENG}
        self.bar_pos = {e: 0 for e in self.ENG}
        self.bar_recs = []
        self.bar_seen = set()

    def sb(self, shape, dt=F32, name=None):
        self.n_t += 1
        return self.es.enter_context(self.nc.sbuf_tensor(name or f"sb{self.n_t}", list(shape), dt))

    def ps(self, shape, dt=F32, name=None):
        self.n_t += 1
        return self.es.enter_context(self.nc.psum_tensor(name or f"ps{self.n_t}", list(shape), dt))

    def scope(self):
        kb = self

        class _S:
            def __enter__(s_):
                s_.old = kb.es
                kb.es = ExitStack()
                return s_

            def __exit__(s_, *a):
                kb.es.close()
                kb.es = s_.old
                kb.barrier()
                return False
        return _S()

    def barrier(self):
        recs = []
        for e in self.ENG:
            last = None
            for r in self.q[e][self.bar_pos[e]:]:
                if r["dma"]:
                    recs.append(r)
                else:
                    last = r
            if last is not None:
                recs.append(last)
            self.bar_pos[e] = len(self.q[e])
        for r in recs:
            r["signal"] = True
        new_eng = {r["eng"] for r in recs if not r["dma"]}
        self.bar_recs = recs + [r for r in self.bar_recs if r["eng"] not in new_eng]
        self.bar_seen = set()

    def dram(self, name, shape, dt=F32, kind="Internal"):
        return self.nc.dram_tensor(name, list(shape), dt, kind=kind)

    def _res(self, x, key):
        if isinstance(x, tuple):
            x, key = x
        if isinstance(x, str):
            n = x
        elif hasattr(x, "tensor"):
            n = x.tensor.name
        else:
            n = x.name
        if isinstance(key, dict):
            key = key.get(n)
        return (n, key)

    def op(self, eng, fn, R=(), W=(), dma=False, rk=None, wk=None):
        rec = {"eng": eng, "fn": fn, "deps": [], "signal": False, "dma": dma, "tok": None}
        deps = []
        Rr = [self._res(r, rk) for r in R if r is not None and not isinstance(r, (int, float))]
        Ww = [self._res(w, wk) for w in W]
        for (t, k) in Rr:
            lw = self.lastw.get(t)
            if lw:
                if k is None:
                    deps.extend(lw.values())
                else:
                    if k in lw: deps.append(lw[k])
                    if None in lw: deps.append(lw[None])
        for (t, k) in Ww:
            lw = self.lastw.get(t)
            rd = self.readers.get(t)
            if lw:
                if k is None:
                    deps.extend(lw.values())
                else:
                    if k in lw: deps.append(lw[k])
                    if None in lw: deps.append(lw[None])
            if rd:
                if k is None:
                    for l in rd.values(): deps.extend(l)
                else:
                    if k in rd: deps.extend(rd[k])
                    if None in rd: deps.extend(rd[None])
        if self.bar_recs and eng not in self.bar_seen:
            self.bar_seen.add(eng)
            for d in self.bar_recs:
                if not (d["eng"] == eng and not d["dma"] and eng == "pe"):
                    rec["deps"].append(d)
        seen = set(id(d) for d in rec["deps"])
        for d in deps:
            if d is rec or id(d) in seen:
                continue
            seen.add(id(d))
            if d["eng"] == "pe" and eng == "pe" and not d["dma"] and not dma:
                continue
            d["signal"] = True
            rec["deps"].append(d)
        for (t, k) in Rr:
            self.readers.setdefault(t, {}).setdefault(k, []).append(rec)
        for (t, k) in Ww:
            if k is None:
                self.lastw[t] = {None: rec}
                self.readers[t] = {}
            else:
                self.lastw.setdefault(t, {})[k] = rec
                rd = self.readers.setdefault(t, {})
                rd[k] = []
        if dma:
            rec["signal"] = True
        self.q[eng].append(rec)
        return rec

    def emit(self):
        nc = self.nc
        sems = {}
        for e in self.ENG:
            n_sig = sum(1 for r in self.q[e] if r["signal"] and not r["dma"])
            n_gen = max(1, -(-n_sig // SEM_GEN))
            gens = [self.es.enter_context(nc.semaphore(f"s_{e}_{g}")) for g in range(n_gen)]
            dsem = [self.es.enter_context(nc.semaphore(f"d_{e}_{g}")) for g in range(N_DMA_SEMS)] \
                if any(r["dma"] for r in self.q[e]) else []
            c = 0
            dc = 0
            for r in self.q[e]:
                if r["dma"]:
                    slot = dc % N_DMA_SEMS
                    r["tok"] = (dsem[slot], 16 * (dc // N_DMA_SEMS + 1), 16)
                    r["prev"] = (dsem[slot], 16 * (dc // N_DMA_SEMS))
                    dc += 1
                elif r["signal"]:
                    g = c // SEM_GEN
                    r["tok"] = (gens[g], c % SEM_GEN + 1, 1)
                    c += 1
        engobj = {"pe": nc.tensor, "dve": nc.vector, "act": nc.scalar, "pool": nc.gpsimd, "sp": nc.sync}
        block = self.es.enter_context(nc.Block())

        def run(e, eo):
            waited = {}
            for r in self.q[e]:
                need = {}
                for d in r["deps"]:
                    s, v, _ = d["tok"]
                    if waited.get(id(s), (None, 0))[1] < v:
                        if id(s) not in need or need[id(s)][1] < v:
                            need[id(s)] = (s, v)
                if r["dma"] and r["prev"][1] > 0:
                    s, v = r["prev"]
                    if waited.get(id(s), (None, 0))[1] < v and (id(s) not in need or need[id(s)][1] < v):
                        need[id(s)] = (s, v)
                for s, v in need.values():
                    eo.wait_ge(s, v)
                    waited[id(s)] = (s, v)
                inst = r["fn"](eo)
                if r["signal"]:
                    s, v, inc = r["tok"]
                    inst.then_inc(s, inc)
            for r in self.q[e]:
                if r["dma"]:
                    s, v, _ = r["tok"]
                    if waited.get(id(s), (None, 0))[1] < v:
                        eo.wait_ge(s, v)
                        waited[id(s)] = (s, v)

        if self.q["sp"]:
            @block.sync
            def _(eo):
                run("sp", eo)
        if self.q["act"]:
            @block.scalar
            def _(eo):
                run("act", eo)
        if self.q["pool"]:
            @block.gpsimd
            def _(eo):
                run("pool", eo)
        if self.q["dve"]:
            @block.vector
            def _(eo):
                run("dve", eo)
        if self.q["pe"]:
            @block.tensor
            def _(eo):
                run("pe", eo)

    def close(self):
        self.es.close()

    def dma(self, out, in_, eng="sp", rk=None, wk=None, **kw):
        return self.op(eng, lambda e: e.dma_start(out=out, in_=in_, **kw), R=[in_], W=[out], dma=True, rk=rk, wk=wk)

    def mm(self, out, lhsT, rhs, start=True, stop=True, rk=None, wk=None, R=(), **kw):
        return self.op("pe", lambda e: e.matmul(out, lhsT, rhs, start=start, stop=stop, **kw),
                       R=[lhsT, rhs] + list(R), W=[out], rk=rk, wk=wk)

    def tr(self, out, in_, ident, rk=None, wk=None):
        return self.op("pe", lambda e: e.transpose(out, in_, ident), R=[in_, ident], W=[out], rk=rk, wk=wk)

    def act(self, out, in_, func, eng="act", rk=None, wk=None, R=(), **kw):
        extra = [v for v in (kw.get("bias"), kw.get("scale"), kw.get("accum_out")) if hasattr(v, "tensor")]
        Wl = [out] + ([kw["accum_out"]] if kw.get("accum_out") is not None else [])
        return self.op(eng, lambda e: e.activation(out=out, in_=in_, func=func, **kw),
                       R=[in_] + extra + list(R), W=Wl, rk=rk, wk=wk)

    def ts(self, eng, out, in0, s1, s2, op0, op1=None, rk=None, wk=None, R=(), **kw):
        extra = [v for v in (s1, s2) if hasattr(v, "tensor")]
        if op1 is None:
            return self.op(eng, lambda e: e.tensor_scalar(out, in0, s1, None, op0, **kw),
                           R=[in0] + extra + list(R), W=[out], rk=rk, wk=wk)
        return self.op(eng, lambda e: e.tensor_scalar(out, in0, s1, s2, op0, op1, **kw),
                       R=[in0] + extra + list(R), W=[out], rk=rk, wk=wk)

    def tt(self, eng, out, in0, in1, op, rk=None, wk=None, R=()):
        return self.op(eng, lambda e: e.tensor_tensor(out, in0, in1, op), R=[in0, in1] + list(R), W=[out], rk=rk, wk=wk)

    def stt(self, out, in0, scalar, in1, op0, op1, eng="dve", rk=None, wk=None, R=()):
        extra = [scalar] if hasattr(scalar, "tensor") else []
        return self.op(eng, lambda e: e.scalar_tensor_tensor(out, in0, scalar, in1, op0, op1),
                       R=[in0, in1] + extra + list(R), W=[out], rk=rk, wk=wk)

    def copy(self, eng, out, in_, rk=None, wk=None):
        if eng == "act":
            return self.op(eng, lambda e: e.copy(out, in_), R=[in_], W=[out], rk=rk, wk=wk)
        return self.op(eng, lambda e: e.tensor_copy(out, in_), R=[in_], W=[out], rk=rk, wk=wk)

    def memset(self, eng, ap, val, wk=None):
        return self.op(eng, lambda e: e.memset(ap, val), W=[ap], wk=wk)

    def scan(self, out, d0, d1, init, op0, op1, rk=None, wk=None):
        extra = [init] if hasattr(init, "tensor") else []
        return self.op("dve", lambda e: e.tensor_tensor_scan(out, d0, d1, init, op0, op1),
                       R=[d0, d1] + extra, W=[out], rk=rk, wk=wk)

    def recip(self, out, in_, eng="dve", rk=None, wk=None):
        return self.op(eng, lambda e: e.reciprocal(out, in_), R=[in_], W=[out], rk=rk, wk=wk)

    def aselect(self, t, pattern, cmp, base, cm, fill=0.0):
        return self.op("pool", lambda e: e.affine_select(t, t, pattern, cmp, fill, base=base, channel_multiplier=cm),
                       R=[t], W=[t])

    def iota(self, t, pattern, base, cm):
        return self.op("pool", lambda e: e.iota(t, pattern, base=base, channel_multiplier=cm), W=[t])

    def gen(self, eng, fn, R=(), W=(), rk=None, wk=None):
        return self.op(eng, fn, R=R, W=W, rk=rk, wk=wk)


D = 2048; KC = 16; NCORE = 8
LC = 256; LL = 8192; T = LC + LL
BR = 512
TWO_PI_SAFE = 6.283185


def _mk(name_shapes_in, name_shapes_out, nc):
    ins = {n: nc.dram_tensor(n, list(s), F32, kind="ExternalInput").ap() for n, s in name_shapes_in.items()}
    outs = {n: nc.dram_tensor(n, list(s), F32, kind="ExternalOutput").ap() for n, s in name_shapes_out.items()}
    return ins, outs


def frac_sin(k, out, y, tmp_i, tmp_f, eng="dve", wk=None):
    k.copy(eng, tmp_i, y)
    k.copy(eng, tmp_f, tmp_i)
    k.tt(eng, tmp_f, y, tmp_f, ALU.subtract)
    k.act(out, tmp_f, AF.Sin, scale=TWO_PI_SAFE)


def build_M(NTK=1024):
    nc = bass.Bass("TRN2", target_bir_lowering=False)
    k = KB(nc)
    NT = 12
    I, O = _mk({"wm": [2, 128, KC, 1536], "bm": [2, 128, NT], "cc": [128, KC, 2],
                "xT": [128, KC, NTK], "tokpos": [2, NTK]},
               {"mT": [2, 128, NT, 2], "x0T": [128, KC, NTK]}, nc)
    cc = k.sb([128, KC, 2]); sc = k.sb([128, KC, 2]); bm = k.sb([128, 2, NT])
    k.dma(cc[:], I["cc"]); k.act(sc[:], cc[:], AF.Silu)
    for l in range(2):
        k.dma(bm[:, l, :], I["bm"][l], eng="act", wk=l)
    wmb = k.sb([128, KC, 1536])
    mo = k.sb([128, 2, NT, 2])
    pss = [k.ps([128, 2]) for _ in range(2)]
    for l in range(2):
        for g in range(4):
            k.dma(wmb[:, 4 * g:4 * g + 4, :], I["wm"][l, :, 4 * g:4 * g + 4, :], eng=("sp" if g % 2 == 0 else "act"), wk=g)
        for t in range(NT):
            ps = pss[t % 2]
            for kc in range(KC):
                k.mm(ps[:], wmb[:, kc, t * 128:(t + 1) * 128], sc[:, kc, :], start=(kc == 0), stop=(kc == KC - 1),
                     rk={wmb.name: kc // 4})
            k.ts("dve", mo[:, l, t, :], ps[:], bm[:, l, t:t + 1], None, ALU.add, wk=(l, t), rk={bm.name: l})
        k.dma(O["mT"][l], mo[:, l], eng="pool", rk=None)
    fi_i = k.sb([128, KC], I32); fi = k.sb([128, KC]); om = k.sb([128, KC]); ph = k.sb([128, KC])
    k.iota(fi_i[:], [[0, 4], [128, 4]], 0, 1)
    k.copy("dve", fi[:], fi_i[:])
    k.act(om[:], fi[:], AF.Exp, scale=-float(np.log(10000.0)) / 512.0)
    k.ts("dve", om[:], om[:], float(1.0 / (2.0 * np.pi)), None, ALU.mult)
    k.memset("dve", ph[:], 0.0)
    k.memset("dve", ph[:, 4:8], 0.25); k.memset("dve", ph[:, 12:16], 0.25)
    posb = k.sb([128, 2, NTK])
    k.dma(posb[:], I["tokpos"].partition_broadcast(128))
    xs = [k.sb([128, NTK]) for _ in range(2)]; ys = [k.sb([128, NTK]) for _ in range(2)]
    yi = [k.sb([128, NTK], I32) for _ in range(2)]; yf = [k.sb([128, NTK]) for _ in range(2)]
    for kc in range(KC):
        b = kc % 2
        k.dma(xs[b][:], I["xT"][:, kc, :], eng=("sp" if b == 0 else "act"))
        k.ts("dve", ys[b][:], posb[:, 0 if kc < 8 else 1, :], om[:, kc:kc + 1], ph[:, kc:kc + 1], ALU.mult, ALU.add)
        frac_sin(k, ys[b][:], ys[b][:], yi[b][:], yf[b][:])
        k.tt("dve", xs[b][:], xs[b][:], ys[b][:], ALU.add)
        k.dma(O["x0T"][:, kc, :], xs[b][:], eng="pool")
    k.emit(); k.close()
    return nc


def fm(a):
    a = np.asarray(a)
    lead = a.shape[:-1]
    b = a.reshape(lead + (KC, 128))
    nd = b.ndim
    perm = (nd - 1, nd - 2) + tuple(range(nd - 2))
    return np.ascontiguousarray(b.transpose(perm))


def run_M(inp):
    LLx = inp["x"].shape[1]
    NTK = LLx // NCORE
    ncm = build_M(NTK)
    w_mod = inp["w_mod"]; b_mod = inp["b_mod"]
    cc = np.ascontiguousarray(np.stack([fm(inp["c"][0]), fm(inp["c_ctx"])], axis=-1))
    maps = []
    for j in range(NCORE):
        wm = w_mod[:, :, j * 1536:(j + 1) * 1536].reshape(2, KC, 128, 1536).transpose(0, 2, 1, 3)
        bm = b_mod[:, j * 1536:(j + 1) * 1536].reshape(2, 12, 128).transpose(0, 2, 1)
        xT = fm(inp["x"][0, j * NTK:(j + 1) * NTK, :])
        t = np.arange(j * NTK, (j + 1) * NTK)
        tokpos = np.stack([(t // 64), (t % 64)]).astype(np.float32)
        maps.append({"wm": np.ascontiguousarray(wm), "bm": np.ascontiguousarray(bm), "cc": cc,
                     "xT": xT, "tokpos": tokpos})
    res = run_bass_kernel_spmd(ncm, maps, core_ids=list(range(NCORE))).results
    m = np.zeros((2, 2, 6 * D), np.float32)
    for j in range(NCORE):
        mT = res[j]["mT"]
        m[:, :, j * 1536:(j + 1) * 1536] = mT.transpose(0, 3, 2, 1).reshape(2, 2, 1536)
    x0T = np.concatenate([res[j]["x0T"] for j in range(NCORE)], axis=2)
    return m, x0T


def a_colspec(j):
    g = j // 2; h = j // 2; d = j % 2
    tiles = [
        [(128 * g, 128)],
        [(512 + 64 * j, 64), (1024 + 64 * j, 64)],
        [(1536 + 64 * j, 64), (5120 + 64 * j, 64)],
        [(2048, 128)], [(2176, 128)], [(2304, 128)], [(2432, 128)],
        [(2560 + 128 * h, 128)],
        [(3072 + 128 * h, 128)],
        [(4096 + 64 * j, 64), (4096 + 64 * j, 64)],
        [(4608 + 64 * j, 64), (5632 + 64 * j, 64)],
        [(3584 + 128 * h, 128)],
    ] + [[(6144 + 1024 * j + 128 * i, 128)] for i in range(8)]
    return tiles

NCT = 20
HG_DEBUG = None
NG0 = 12
P_A, P_R, P_K, P_V, P_LX, P_X, P_Q, P_F, P_LG = 0, 128, 192, 256, 320, 384, 896, 1024, 1152
P_F2 = 1216
P_ROWS = 1344


def seq_tiles(LCx, LLx, n):
    out = []
    for (s0, L, c) in ((0, LCx, 1), (LCx, LLx, 0)):
        t = 0
        while t < L:
            m = min(n, L - t)
            out.append((s0 + t, m, c))
            t += m
    return out


def a_stage_proj(k, I, O, P, Vi, LCx, LLx):
    with k.scope():
        vec = k.sb([128, KC, 5]); k.dma(vec[:], I["vecD"])
        g1 = k.sb([128, KC, 2]); shf = k.sb([128, KC, 2])
        for s in range(2):
            k.ts("dve", g1[:, :, s], vec[:, :, 2 + 2 * s], 1.0, None, ALU.add, wk=s)
            k.tt("dve", g1[:, :, s], g1[:, :, s], vec[:, :, 0], ALU.mult, wk=s, rk={g1.name: s})
            k.copy("dve", shf[:, :, s], vec[:, :, 1 + 2 * s], wk=s)
        ones = k.sb([128, 128]); k.memset("dve", ones[:], 1.0 / D)
        xb = [k.sb([128, KC, 512]) for _ in range(2)]
        sq = k.sb([128, KC, 512])
        hT = [k.sb([128, KC, 512]) for _ in range(2)]
        rstd = k.sb([128, 512])
        wbuf = [k.sb([128, KC, 128]) for _ in range(3)]
        stg = [k.sb([128, 512]) for _ in range(3)]
        pn = k.ps([128, 512])
        pp = [k.ps([128, 512]) for _ in range(3)]
        wi = 0; si = 0
        for ti, (t0, n, s) in enumerate(seq_tiles(LCx, LLx, 512)):
            b = ti % 2
            k.dma(xb[b][:, 0:8, :n], I["xT"][:, 0:8, t0:t0 + n], eng="sp", wk=0)
            k.dma(xb[b][:, 8:16, :n], I["xT"][:, 8:16, t0:t0 + n], eng="act", wk=1)
            k.act(sq[:, :, :n], xb[b][:, :, :n], AF.Square)
            for kc in range(KC):
                k.mm(pn[:, :n], ones[:], sq[:, kc, :n], start=(kc == 0), stop=(kc == KC - 1))
            k.ts("dve", rstd[:, :n], pn[:, :n], 1e-6, None, ALU.add)
            k.act(rstd[:, :n], rstd[:, :n], AF.Sqrt)
            k.recip(rstd[:, :n], rstd[:, :n])
            h = hT[b]
            k.tt("dve", h[:, :, :n], xb[b][:, :, :n], rstd[:, :n].unsqueeze(1).to_broadcast([128, KC, n]), ALU.mult)
            k.tt("pool", h[:, :, :n], h[:, :, :n], g1[:, :, s:s + 1].to_broadcast([128, KC, n]), ALU.mult)
            k.tt("dve", h[:, :, :n], h[:, :, :n], shf[:, :, s:s + 1].to_broadcast([128, KC, n]), ALU.add)
            for ct in range(NCT):
                wb = wbuf[wi % 3]; wi += 1
                k.dma(wb[:, 0:8, :], I["wA"][ct, :, 0:8, :], eng="sp", wk=0)
                k.dma(wb[:, 8:16, :], I["wA"][ct, :, 8:16, :], eng="act", wk=1)
                if ct == 9:
                    for sub in range(n // 128):
                        ps = pp[si % 3]; st = stg[si % 3]; si += 1
                        for kc in range(KC):
                            k.mm(ps[:, :128], h[:, kc, sub * 128:(sub + 1) * 128], wb[:, kc, :],
                                 start=(kc == 0), stop=(kc == KC - 1))
                        k.copy("dve", st[:, :128], ps[:, :128])
                        k.dma(Vi[t0 + sub * 128:t0 + (sub + 1) * 128, :], st[:, :128], eng="pool", wk=ti)
                    continue
                ps = pp[si % 3]; st = stg[si % 3]; si += 1
                for kc in range(KC):
                    k.mm(ps[:, :n], wb[:, kc, :], h[:, kc, :n], start=(kc == 0), stop=(kc == KC - 1))
                if ct >= NG0:
                    k.act(st[:, :n], ps[:, :n], AF.Sigmoid)
                    k.dma(O["gT"][(ct - NG0) * 128:(ct - NG0 + 1) * 128, t0:t0 + n], st[:, :n], eng="pool")
                elif ct == 10:
                    k.copy("dve", st[:, :n], ps[:, :n])
                    k.dma(O["ogT"][:, t0:t0 + n], st[0:64, :n], eng="pool")
                    k.dma(P[P_LG:P_LG + 64, t0:t0 + n], st[64:128, :n], eng="pool", wk=ti)
                else:
                    row = {0: P_A, 1: P_R, 2: P_V, 3: P_X, 4: P_X + 128, 5: P_X + 256, 6: P_X + 384, 7: P_Q, 8: P_F, 11: P_F2}[ct]
                    if ct % 2:
                        k.copy("dve", st[:, :n], ps[:, :n])
                    else:
                        k.copy("act", st[:, :n], ps[:, :n])
                    k.dma(P[row:row + 128, t0:t0 + n], st[:, :n], eng="pool", wk=ti)
    k.barrier()


A_IN = lambda Tx: {"xT": [128, KC, Tx], "wA": [NCT, 128, KC, 128], "vecD": [128, KC, 5],
                   "rwv": [64, 16], "rwx": [128, 4, 3], "rw1": [128, 4, 3, 64], "rw2": [64, 3, 64],
                   "lruv": [64, 16], "lruw": [64, 4, 64], "hgv": [128, 2], "cprime": [1, 64]}
A_OUT = lambda Tx: {"gT": [1024, Tx], "ogT": [64, Tx], "yA": [64, Tx], "yB": [64, Tx], "yC": [64, Tx],
                    "yD": [64, Tx]}


def build_A(layer, LCx=LC, LLx=LL, stages=("proj", "fnet", "lru", "hgrn", "rwkv"), debug=False):
    Tx = LCx + LLx
    nc = bass.Bass("TRN2", target_bir_lowering=False)
    k = KB(nc)
    outs = dict(A_OUT(Tx))
    if debug:
        outs["Pdbg"] = [P_ROWS, Tx]; outs["Vdbg"] = [Tx, 128]
    I, O = _mk(A_IN(Tx), outs, nc)
    if debug:
        P = O["Pdbg"]; Vi = O["Vdbg"]
    else:
        P = k.dram("Pscr", [P_ROWS, Tx]).ap(); Vi = k.dram("Viscr", [Tx, 128]).ap()
    if "proj" in stages:
        a_stage_proj(k, I, O, P, Vi, LCx, LLx)
    if "fnet" in stages:
        a_stage_fnet(k, I, O, P, LCx, LLx)
    if "lru" in stages:
        a_stage_lru(k, I, O, P, LCx, LLx)
    if "hgrn" in stages:
        a_stage_hgrn(k, I, O, P, Vi, LCx, LLx, layer)
    if "rwkv" in stages:
        a_stage_rwkv(k, I, O, P, LCx, LLx)
    k.emit(); k.close()
    return nc


def a_host_inputs(inp, l, j, xT_full, m):
    w_in = inp["w_in"][l]
    tiles = a_colspec(j)
    wA = np.empty((NCT, 128, KC, 128), np.float32)
    for ci, pieces in enumerate(tiles):
        cols = np.concatenate([np.arange(s, s + n) for s, n in pieces])
        wA[ci] = w_in[:, cols].reshape(KC, 128, 128).transpose(1, 0, 2)
    mm = m[l].reshape(2, 6, D)
    vecD = np.stack([fm(inp["norm_mix_g"][l]), fm(mm[0, 0]), fm(mm[0, 1]), fm(mm[1, 0]), fm(mm[1, 1])], axis=-1)
    hs = slice(64 * j, 64 * j + 64)
    rwv = np.zeros((64, 16), np.float32)
    mu = inp["rw_mu"][l]
    cols = [mu[0, hs], mu[1, hs], mu[2, hs], inp["rw_w0"][l, 0, hs], inp["rw_w0"][l, 1, hs],
            inp["rw_a0"][l, 0, hs], inp["rw_a0"][l, 1, hs], inp["rw_kk"][l, hs], inp["rw_ka"][l, hs],
            inp["rw_rk"][l, j], inp["rw_ln_g"][l, hs], inp["rw_ln_b"][l, hs]]
    for i, c in enumerate(cols):
        rwv[:, i] = c
    rwx = np.ascontiguousarray(mu[3:6].reshape(3, 4, 128).transpose(2, 1, 0))
    w1 = inp["rw_w1"][l]; a1 = inp["rw_a1"][l]; g1 = inp["rw_g1"][l]
    rw1 = np.stack([np.concatenate([w1[0], w1[1]], axis=1), np.concatenate([a1[0], a1[1]], axis=1), g1], axis=1)
    rw1 = np.ascontiguousarray(rw1.reshape(4, 128, 3, 64).transpose(1, 0, 2, 3))
    w2 = inp["rw_w2"][l]; a2 = inp["rw_a2"][l]; g2 = inp["rw_g2"][l]
    rw2 = np.stack([np.concatenate([w2[0][:, hs], w2[1][:, hs]], axis=0),
                    np.concatenate([a2[0][:, hs], a2[1][:, hs]], axis=0), g2[:, hs]], axis=1)
    lruv = np.zeros((64, 16), np.float32)
    cw = inp["lru_conv_w"][l]
    cols = [cw[0, hs], cw[1, hs], cw[2, hs], cw[3, hs], inp["lru_conv_b"][l, hs],
            inp["lru_ba"][l, 0, hs], inp["lru_ba"][l, 1, hs], inp["lru_bx"][l, 0, hs], inp["lru_bx"][l, 1, hs],
            inp["lru_lam"][l, 0, hs], inp["lru_lam"][l, 1, hs]]
    for i, c in enumerate(cols):
        lruv[:, i] = c
    lruw = np.stack([inp["lru_wa"][l, 0, j], inp["lru_wa"][l, 1, j], inp["lru_wx"][l, 0, j], inp["lru_wx"][l, 1, j]], axis=1)
    h = j // 2
    hgv = np.ascontiguousarray(inp["hg_lb_logits"][:, 128 * h:128 * h + 128].T)
    cprime = (np.arange(64) + 64 * (j % 2)).astype(np.float32)[None, :]
    return {"xT": xT_full, "wA": wA, "vecD": np.ascontiguousarray(vecD), "rwv": rwv, "rwx": rwx, "rw1": rw1,
            "rw2": np.ascontiguousarray(rw2), "lruv": lruv, "lruw": np.ascontiguousarray(lruw), "hgv": hgv,
            "cprime": cprime}


def dir_tiles(LCx, LLx, n, d):
    ts_ = seq_tiles(LCx, LLx, n)
    if d == 0:
        return ts_
    c = [t for t in ts_ if t[2] == 1][::-1]
    l_ = [t for t in ts_ if t[2] == 0][::-1]
    return c + l_


def a_stage_lru(k, I, O, P, LCx, LLx):
    Tx = LCx + LLx
    with k.scope():
        lv = k.sb([64, 16]); k.dma(lv[:], I["lruv"])
        lw = k.sb([64, 4, 64]); k.dma(lw[:], I["lruw"], eng="act")
        cd = k.sb([64, 2])
        k.act(cd[:], lv[:, 9:11], AF.Exp, scale=-1.0)
        k.act(cd[:], cd[:], AF.Ln, bias=1.0)
        k.ts("dve", cd[:], cd[:], -8.0, None, ALU.mult)
        u0 = k.sb([64, Tx]); xc = k.sb([64, Tx]); hs = k.sb([64, Tx])
        k.dma(u0[:], P[P_LX:P_LX + 64, :])
        for (s0, L) in ((0, LCx), (LCx, LLx)):
            k.ts("dve", xc[:, s0:s0 + L], u0[:, s0:s0 + L], lv[:, 2:3], lv[:, 4:5], ALU.mult, ALU.add)
            k.stt(xc[:, s0 + 2:s0 + L], u0[:, s0:s0 + L - 2], lv[:, 0:1], xc[:, s0 + 2:s0 + L], ALU.mult, ALU.add)
            k.stt(xc[:, s0 + 1:s0 + L], u0[:, s0:s0 + L - 1], lv[:, 1:2], xc[:, s0 + 1:s0 + L], ALU.mult, ALU.add)
            k.stt(xc[:, s0:s0 + L - 1], u0[:, s0 + 1:s0 + L], lv[:, 3:4], xc[:, s0:s0 + L - 1], ALU.mult, ALU.add)
        pr = [k.ps([64, 512]) for _ in range(2)]; pi = [k.ps([64, 512]) for _ in range(2)]
        rr = [k.sb([64, 512]) for _ in range(2)]; ii = [k.sb([64, 512]) for _ in range(2)]
        aa = [k.sb([64, 512]) for _ in range(2)]; mmv = [k.sb([64, 512]) for _ in range(2)]
        hh = [k.sb([64, 512]) for _ in range(2)]
        carry = k.sb([64, 2])
        for d in range(2):
            first = True
            for ti, (t0, n, c) in enumerate(dir_tiles(LCx, LLx, 512, d)):
                b = ti % 2
                xs = xc[:, t0:t0 + n]
                k.mm(pr[b][:, :n], lw[:, d, :], xs)
                k.mm(pi[b][:, :n], lw[:, 2 + d, :], xs)
                k.act(rr[b][:, :n], pr[b][:, :n], AF.Sigmoid, bias=lv[:, 5 + d:6 + d])
                k.act(ii[b][:, :n], pi[b][:, :n], AF.Sigmoid, bias=lv[:, 7 + d:8 + d])
                k.act(aa[b][:, :n], rr[b][:, :n], AF.Exp, scale=cd[:, d:d + 1])
                k.tt("pool", mmv[b][:, :n], aa[b][:, :n], aa[b][:, :n], ALU.mult)
                k.ts("pool", mmv[b][:, :n], mmv[b][:, :n], -1.0, 1.0, ALU.mult, ALU.add)
                k.act(mmv[b][:, :n], mmv[b][:, :n], AF.Sqrt)
                k.tt("pool", ii[b][:, :n], ii[b][:, :n], mmv[b][:, :n], ALU.mult)
                k.tt("dve", ii[b][:, :n], ii[b][:, :n], xs, ALU.mult)
                init = 0.0 if first else carry[:, d:d + 1]
                if d == 0:
                    k.scan(hh[b][:, :n], aa[b][:, :n], ii[b][:, :n], init, ALU.mult, ALU.add)
                    k.copy("dve", carry[:, 0:1], hh[b][:, n - 1:n], wk=0, rk=None)
                    k.copy("pool", hs[:, t0:t0 + n], hh[b][:, :n], wk=(t0))
                else:
                    k.scan(hh[b][:, n - 1::-1] if n < 512 else hh[b][:, ::-1],
                           aa[b][:, n - 1::-1] if n < 512 else aa[b][:, ::-1],
                           ii[b][:, n - 1::-1] if n < 512 else ii[b][:, ::-1], init, ALU.mult, ALU.add)
                    k.copy("dve", carry[:, 1:2], hh[b][:, 0:1], wk=1, rk=None)
                    k.tt("pool", hs[:, t0:t0 + n], hs[:, t0:t0 + n], hh[b][:, :n], ALU.add, wk=(t0), rk={hs.name: t0})
                first = False
        k.dma(u0[:], P[P_LG:P_LG + 64, :])
        x2 = [k.sb([64, 2048]) for _ in range(2)]
        for ti, t0 in enumerate(range(0, Tx, 2048)):
            n = min(2048, Tx - t0); b = ti % 2
            xs = u0[:, t0:t0 + n]; w = x2[b][:, :n]
            k.act(w, xs, AF.Square)
            k.ts("dve", w, w, 0.044715, 1.0, ALU.mult, ALU.add)
            k.tt("dve", w, w, xs, ALU.mult)
            k.act(w, w, AF.Tanh, scale=0.7978845608028654)
            k.ts("dve", w, w, 0.5, 0.5, ALU.mult, ALU.add)
            k.tt("pool", w, w, xs, ALU.mult)
            k.tt("dve", w, w, hs[:, t0:t0 + n], ALU.mult)
            k.dma(O["yD"][:, t0:t0 + n], w, eng="act")


def a_stage_hgrn(k, I, O, P, Vi, LCx, LLx, layer):
    Tx = LCx + LLx
    with k.scope():
        hv = k.sb([128, 2]); k.dma(hv[:], I["hgv"])
        lb = k.sb([128, 1]); oml = k.sb([128, 1])
        if layer == 0:
            k.memset("dve", lb[:], 0.0); k.memset("dve", oml[:], 1.0)
        else:
            k.tt("dve", lb[:], hv[:, 1:2], hv[:, 0:1], ALU.subtract)
            k.act(lb[:], lb[:], AF.Sigmoid)
            k.ts("dve", oml[:], lb[:], -1.0, 1.0, ALU.mult, ALU.add)
        ident = k.sb([128, 128]); k.memset("dve", ident[:], 1.0)
        k.aselect(ident[:], [[-1, 128]], ALU.is_equal, 0, 1)
        M8 = k.sb([128, 8]); k.memset("dve", M8[:], 1.0)
        k.aselect(M8[:], [[-16, 8]], ALU.is_ge, 0, 1)
        k.aselect(M8[:], [[16, 8]], ALU.is_ge, 15, -1)
        MASK = [k.sb([128, 128]) for _ in range(2)]
        for d in range(2):
            mk_ = MASK[d]
            k.memset("dve", mk_[:], 1.0)
            k.aselect(mk_[:], [[-16, 8], [0, 16]], ALU.is_ge, 0, 1)
            k.aselect(mk_[:], [[16, 8], [0, 16]], ALU.is_ge, 15, -1)
            if d == 0:
                k.aselect(mk_[:], [[16, 8], [1, 16]], ALU.is_ge, 0, -1)
            else:
                k.aselect(mk_[:], [[-16, 8], [-1, 16]], ALU.is_ge, 0, 1)
        cm = [k.sb([128, 512]) for _ in range(2)]
        k.memset("dve", cm[0][:], 1.0); k.memset("dve", cm[0][:, 0::16], 0.0)
        k.memset("dve", cm[1][:], 1.0); k.memset("dve", cm[1][:, 15::16], 0.0)
        ysum = k.sb([64, Tx]); k.memset("pool", ysum[:], 0.0)
        S = [k.sb([128, 128]) for _ in range(2)]
        for d in range(2):
            k.memset("dve", S[d][:], 0.0)
        nb = 2
        zt = [[k.sb([128, 512]) for _ in range(nb)] for d in range(2)]
        qt = [[k.sb([128, 512]) for _ in range(nb)] for d in range(2)]
        kh = [[k.sb([128, 512]) for _ in range(nb)] for d in range(2)]
        Gt = [[k.sb([128, 512]) for _ in range(nb)] for d in range(2)]
        kb_ = [[k.sb([128, 512]) for _ in range(nb)] for d in range(2)]
        eGl = [[k.sb([128, 32]) for _ in range(nb)] for d in range(2)]
        Vt = [[k.sb([128, 4, 128]) for _ in range(nb)] for d in range(2)]
        Ktm = [k.sb([128, 128]) for d in range(2)]
        Km = [k.sb([128, 8, 128]) for d in range(2)]
        Am = [k.sb([128, 128]) for d in range(2)]
        Xs = [k.sb([128, 128]) for d in range(2)]
        pb1 = [k.ps([128, 512]) for d in range(2)]
        pb2 = [k.ps([128, 512]) for d in range(2)]
        pb3 = [k.ps([128, 512]) for d in range(2)]
        blocks = [dir_tiles(LCx, LLx, 512, d) for d in range(2)]
        nblk = len(blocks[0])

        def pre(d, bi):
            t0, n, c = blocks[d][bi]
            b = bi % nb
            z = zt[d][b][:, :n]; q = qt[d][b][:, :n]; kk_ = kh[d][b][:, :n]; G = Gt[d][b][:, :n]; KB_ = kb_[d][b][:, :n]
            nch = n // 16
            k.dma(z, P[(P_F if d == 0 else P_F2):(P_F if d == 0 else P_F2) + 128, t0:t0 + n], eng="sp")
            k.dma(q, P[P_Q:P_Q + 128, t0:t0 + n], eng="act")
            k.dma(Vt[d][b][:, :n // 128, :], Vi[t0:t0 + n, :].rearrange("(a s) c -> s a c", s=128), eng="pool")
            k.act(z, z, AF.Sigmoid)
            k.ts("dve", z, z, oml[:, 0:1], lb[:, 0:1], ALU.mult, ALU.add)
            k.act(G, z, AF.Ln)
            k.ts("pool", z, z, -1.0, 1.0, ALU.mult, ALU.add)
            if d == 0:
                k.scan(G, cm[0][:, :n], G, 0.0, ALU.mult, ALU.add)
            else:
                k.scan(Gt[d][b][:, n - 1::-1] if n < 512 else Gt[d][b][:, ::-1],
                       cm[1][:, n - 1::-1] if n < 512 else cm[1][:, ::-1],
                       Gt[d][b][:, n - 1::-1] if n < 512 else Gt[d][b][:, ::-1], 0.0, ALU.mult, ALU.add)
            k.act(q, q, AF.Silu)
            k.act(kk_, G, AF.Exp)
            k.tt("dve", q, q, kk_, ALU.mult)
            k.act(kk_, G, AF.Exp, scale=-1.0)
            k.tt("dve", kk_, kk_, z, ALU.mult)
            gl = Gt[d][b][:, 15:n:16] if d == 0 else Gt[d][b][:, 0:n:16]
            k.act(eGl[d][b][:, :nch], gl, AF.Exp)
            k.tt("pool", kb_[d][b][:, :n].rearrange("p (c i) -> p c i", i=16),
                 kh[d][b][:, :n].rearrange("p (c i) -> p c i", i=16),
                 eGl[d][b][:, :nch].unsqueeze(2).to_broadcast([128, nch, 16]), ALU.mult)

        def chain(d, bi):
            t0, n, c = blocks[d][bi]
            b = bi % nb
            nt = n // 128
            subs = range(nt) if d == 0 else range(nt - 1, -1, -1)
            for sub in subs:
                cs = slice(sub * 128, (sub + 1) * 128)
                pT = pb1[d][:, 0:128]; pA = pb1[d][:, 128:256]; pU = pb3[d][:, 0:128]
                pX = pb2[d][:, 0:128]; pY = pb2[d][:, 128:256]
                k.tr(pT, kb_[d][b][:, cs], ident[:])
                k.copy("act", Ktm[d][:], pT)
                k.tt("pool", Km[d][:], Ktm[d][:].unsqueeze(1).to_broadcast([128, 8, 128]),
                     M8[:].unsqueeze(2).to_broadcast([128, 8, 128]), ALU.mult)
                k.mm(pA, kh[d][b][:, cs], qt[d][b][:, cs])
                k.tt("dve", Am[d][:], pA, MASK[d][:], ALU.mult)
                k.mm(pX, Vt[d][b][:, sub, :], Am[d][:])
                k.copy("act", Xs[d][:], pX)
                chs = range(8) if d == 0 else range(7, -1, -1)
                for ch in chs:
                    c0 = sub * 128 + ch * 16
                    k.mm(pb2[d][:, 128 + ch * 16:128 + (ch + 1) * 16], S[d][:], qt[d][b][:, c0:c0 + 16])
                    k.mm(pU, Km[d][:, ch, :], Vt[d][b][:, sub, :])
                    gi = sub * 8 + ch
                    k.stt(S[d][:], S[d][:], eGl[d][b][:, gi:gi + 1], pU, ALU.mult, ALU.add)
                tt0 = t0 + sub * 128
                k.tt("dve", Xs[d][:], Xs[d][:], pY, ALU.add)
                k.tt("pool", ysum[:, tt0:tt0 + 128], ysum[:, tt0:tt0 + 128], Xs[d][0:64, :], ALU.add, wk=tt0, rk={ysum.name: tt0})

        for d in range(2):
            pre(d, 0)
        for bi in range(nblk):
            for d in range(2):
                if bi + 1 < nblk:
                    pre(d, bi + 1)
            for d in range(2):
                if HG_DEBUG != "pre":
                    chain(d, bi)
        k.dma(O["yC"], ysum[:])


def k_frac(k, out, y, ti, tf, e1="pool", e2="dve"):
    k.copy(e1, ti, y)
    k.copy(e1, tf, ti)
    k.tt(e2, out, y, tf, ALU.subtract)


HALF_PI_SAFE = 1.5707962


def a_stage_fnet(k, I, O, P, LCx, LLx):
    with k.scope():
        cp = k.sb([128, 64]); k.dma(cp[:], I["cprime"].broadcast_to([128, 64]))
        pi_i = k.sb([128, 1], I32); pidx = k.sb([128, 1])
        k.iota(pi_i[:], [[0, 1]], 0, 1)
        k.copy("dve", pidx[:], pi_i[:])
        yy = k.sb([128, 64]); yi = k.sb([128, 64], I32); yf = k.sb([128, 64]); CS = k.sb([128, 128])
        k.ts("dve", yy[:], cp[:], pidx[:, 0:1], 1.0 / 128.0, ALU.mult, ALU.mult)
        k_frac(k, yy[:], yy[:], yi[:], yf[:])
        k.act(CS[:, 64:128], yy[:], AF.Sin, scale=-TWO_PI_SAFE)
        k.act(yf[:], yy[:], AF.Abs)
        k.act(CS[:, 0:64], yf[:], AF.Sin, scale=-TWO_PI_SAFE, bias=HALF_PI_SAFE)
        pq_ps = k.ps([128, 128])
        for (s0, L) in ((0, LCx), (LCx, LLx)):
            with k.scope():
                nch = L // 128
                Lh = min(L, 2048)
                TB = min(Lh, 512)
                ntb = Lh // TB
                U = k.sb([128, L]); k.dma(U[:], P[P_A:P_A + 128, s0:s0 + L])
                PQ = k.sb([128, nch, 128])
                for a in range(nch):
                    k.mm(pq_ps[:], U[:, a * 128:(a + 1) * 128], CS[:])
                    k.copy("dve" if a % 2 else "act", PQ[:, a, :], pq_ps[:], wk=a)
                tp_i = k.sb([128, Lh], I32); tp = k.sb([128, Lh])
                f1 = k.sb([128, Lh]); stp = k.sb([128, Lh]); g = k.sb([128, Lh]); tmp = k.sb([128, Lh])
                ti = k.sb([128, Lh], I32); tf = k.sb([128, Lh]); ab = k.sb([128, Lh])
                Ss = [k.sb([128, Lh]) for _ in range(2)]; Cc = [k.sb([128, Lh]) for _ in range(2)]
                Yp = [k.ps([64, 512]) for _ in range(ntb)]
                yo = [k.sb([64, 512]) for _ in range(2)]
                scale = float(1.0 / np.sqrt(L * 128.0))
                for h0 in range(0, L, Lh):
                    k.iota(tp_i[:], [[1, Lh]], h0, 0)
                    k.copy("dve", tp[:], tp_i[:])
                    k.ts("dve", f1[:], tp[:], pidx[:, 0:1], 1.0 / L, ALU.mult, ALU.mult)
                    k_frac(k, g[:], f1[:], ti[:], tf[:])
                    k.ts("dve", stp[:], tp[:], 128.0 / L, None, ALU.mult)
                    k_frac(k, stp[:], stp[:], ti[:], tf[:])
                    for a in range(nch):
                        b = a % 2
                        if a > 0:
                            k.tt("dve", tmp[:], g[:], stp[:], ALU.add)
                            k_frac(k, g[:], tmp[:], ti[:], tf[:])
                        k.act(Ss[b][:], g[:], AF.Sin, scale=TWO_PI_SAFE)
                        k.act(ab[:], g[:], AF.Abs)
                        k.act(Cc[b][:], ab[:], AF.Sin, scale=-TWO_PI_SAFE, bias=HALF_PI_SAFE)
                        for tb in range(ntb):
                            k.mm(Yp[tb][:, :TB], PQ[:, a, 0:64], Cc[b][:, tb * TB:(tb + 1) * TB], start=(a == 0), stop=False,
                                 rk={PQ.name: a})
                            k.mm(Yp[tb][:, :TB], PQ[:, a, 64:128], Ss[b][:, tb * TB:(tb + 1) * TB], start=False,
                                 stop=(a == nch - 1), rk={PQ.name: a})
                    for tb in range(ntb):
                        k.ts("dve", yo[tb % 2][:, :TB], Yp[tb][:, :TB], scale, None, ALU.mult)
                        k.dma(O["yA"][:, s0 + h0 + tb * TB:s0 + h0 + (tb + 1) * TB], yo[tb % 2][:, :TB], eng="sp")


RW_R, RW_KK, RW_W0, RW_W1, RW_NB0, RW_NB1, RW_KT0, RW_KT1, RW_G, RW_BV = range(10)


def a_stage_rwkv(k, I, O, P, LCx, LLx):
    Tx = LCx + LLx
    RW = k.dram("RWscr", [10, 64, Tx]).ap()
    Vtm = k.dram("Vtmscr", [Tx, 64]).ap()
    seq_end = {0: LCx, LCx: Tx}
    with k.scope():
        rv = k.sb([64, 16]); k.dma(rv[:], I["rwv"])
        rx = k.sb([128, 4, 3]); k.dma(rx[:], I["rwx"], eng="act")
        r1 = k.sb([128, 4, 3, 64]); k.dma(r1[:], I["rw1"], eng="act")
        r2 = k.sb([64, 3, 64]); k.dma(r2[:], I["rw2"])
        omka = k.sb([64, 1]); k.ts("dve", omka[:], rv[:, 8:9], -1.0, 1.0, ALU.mult, ALU.add)
        ones64 = k.sb([64, 64]); k.memset("dve", ones64[:], 1.0)
        ident = k.sb([64, 64]); k.memset("dve", ident[:], 1.0)
        k.aselect(ident[:], [[-1, 64]], ALU.is_equal, 0, 1)
        N = 512
        u3 = k.sb([64, 3, N + 2])
        ux = k.sb([128, 4, N + 2])
        t3 = k.sb([64, 3, N]); rkv = k.sb([64, 3, N])
        tx = k.sb([128, 4, N]); xxx = k.sb([128, 4, N]); xm = [k.sb([128, 4, N]) for _ in range(3)]
        lw_ = k.sb([64, N]); la_ = k.sb([64, N]); lg_ = k.sb([64, N])
        wd = [k.sb([64, N]) for _ in range(2)]; ad = [k.sb([64, N]) for _ in range(2)]
        ktd = [k.sb([64, N]) for _ in range(2)]; nbd = [k.sb([64, N]) for _ in range(2)]
        gg = k.sb([64, N]); kkk = k.sb([64, N]); sq = k.sb([64, N]); kkn = k.sb([64, N]); bv = k.sb([64, N])
        vt = k.sb([128, 4, 64])
        pl = [k.ps([64, 512]) for _ in range(3)]; pu = [k.ps([64, 512]) for _ in range(2)]
        pv = k.ps([128, 512])
        for ti_, (t0, n, c) in enumerate(seq_tiles(LCx, LLx, N)):
            s0 = 0 if c == 1 else LCx
            e0 = seq_end[s0]
            lo = t0 - 1 if t0 > s0 else t0
            hi = t0 + n + 1 if t0 + n < e0 else t0 + n
            o = 1 - (t0 - lo)
            if t0 == s0:
                k.memset("dve", u3[:, :, 0:1], 0.0); k.memset("pool", ux[:, :, 0:1], 0.0)
            if t0 + n == e0:
                k.memset("dve", u3[:, :, n + 1:n + 2], 0.0); k.memset("pool", ux[:, :, n + 1:n + 2], 0.0)
            w_ = hi - lo
            k.dma(u3[:, 0:2, o:o + w_], P[P_R:P_R + 128, lo:hi].rearrange("(c p) t -> p c t", p=64), eng="sp")
            k.dma(u3[:, 2, o:o + w_], P[P_V:P_V + 64, lo:hi], eng="act")
            k.dma(ux[:, :, o:o + w_], P[P_X:P_X + 512, lo:hi].rearrange("(c p) t -> p c t", p=128), eng="pool")
            k.tt("dve", t3[:, :, :n], u3[:, :, 0:n], u3[:, :, 2:n + 2], ALU.add)
            k.stt(t3[:, :, :n], t3[:, :, :n], 0.5, u3[:, :, 1:n + 1], ALU.mult, ALU.subtract)
            for i in range(3):
                k.stt(rkv[:, i, :n], t3[:, i, :n], rv[:, i:i + 1], u3[:, i, 1:n + 1], ALU.mult, ALU.add)
            k.tt("pool", tx[:, :, :n], ux[:, :, 0:n], ux[:, :, 2:n + 2], ALU.add)
            k.stt(xxx[:, :, :n], tx[:, :, :n], 0.5, ux[:, :, 1:n + 1], ALU.mult, ALU.subtract)
            for i in range(3):
                k.tt("pool", xm[i][:, :, :n], xxx[:, :, :n], rx[:, :, i:i + 1].to_broadcast([128, 4, n]), ALU.mult)
                k.tt("dve" if i == 1 else "pool", xm[i][:, :, :n], xm[i][:, :, :n], ux[:, :, 1:n + 1], ALU.add)
            for i in range(3):
                for cc_ in range(4):
                    k.mm(pl[i][:, :n], r1[:, cc_, i, :], xm[i][:, cc_, :n], start=(cc_ == 0), stop=(cc_ == 3))
            k.act(lw_[:, :n], pl[0][:, :n], AF.Tanh)
            k.copy("dve", la_[:, :n], pl[1][:, :n])
            k.act(lg_[:, :n], pl[2][:, :n], AF.Sigmoid)
            R_, K_, V_ = rkv[:, 0, :n], rkv[:, 1, :n], rkv[:, 2, :n]
            for d in range(2):
                k.mm(pu[0][:, :n], r2[32 * d:32 * d + 32, 0, :], lw_[32 * d:32 * d + 32, :n])
                k.act(wd[d][:, :n], pu[0][:, :n], AF.Sigmoid, bias=rv[:, 3 + d:4 + d])
                k.act(wd[d][:, :n], wd[d][:, :n], AF.Exp, scale=-float(np.exp(-0.5)))
                k.mm(pu[1][:, :n], r2[32 * d:32 * d + 32, 1, :], la_[32 * d:32 * d + 32, :n])
                k.act(ad[d][:, :n], pu[1][:, :n], AF.Sigmoid, bias=rv[:, 5 + d:6 + d])
            k.mm(pu[0][:, :n], r2[:, 2, :], lg_[:, :n])
            k.copy("act", gg[:, :n], pu[0][:, :n])
            k.ts("dve", kkk[:, :n], K_, rv[:, 7:8], None, ALU.mult)
            k.act(sq[:, :n], kkk[:, :n], AF.Square)
            k.mm(pu[1][:, :n], ones64[:], sq[:, :n])
            k.act(sq[:, :n], pu[1][:, :n], AF.Sqrt)
            k.ts("dve", sq[:, :n], sq[:, :n], 1e-12, None, ALU.max)
            k.recip(sq[:, :n], sq[:, :n])
            k.tt("dve", kkn[:, :n], kkk[:, :n], sq[:, :n], ALU.mult)
            for d in range(2):
                k.ts("dve", ktd[d][:, :n], ad[d][:, :n], rv[:, 8:9], omka[:, 0:1], ALU.mult, ALU.add)
                k.tt("dve", ktd[d][:, :n], ktd[d][:, :n], K_, ALU.mult)
                k.stt(nbd[d][:, :n], ad[d][:, :n], -1.0, kkn[:, :n], ALU.mult, ALU.mult)
            k.tt("pool", bv[:, :n], ktd[0][:, :n], ktd[1][:, :n], ALU.add)
            k.stt(bv[:, :n], bv[:, :n], rv[:, 9:10], R_, ALU.mult, ALU.mult)
            k.mm(pu[0][:, :n], ones64[:], bv[:, :n])
            k.tt("dve", bv[:, :n], pu[0][:, :n], V_, ALU.mult)
            for sub in range(n // 128):
                k.tr(pv[:, sub * 64:(sub + 1) * 64], rkv[:, 2, sub * 128:(sub + 1) * 128], ident[:])
            k.copy("act", vt[:, :n // 128, :], pv[:, :(n // 128) * 64].rearrange("p (a c) -> p a c", c=64))
            k.dma(Vtm[t0:t0 + n, :].rearrange("(a s) c -> s a c", s=128), vt[:, :n // 128, :], eng="sp", wk=ti_)
            for idx, src in ((RW_R, R_), (RW_KK, kkn[:, :n]), (RW_W0, wd[0][:, :n]), (RW_W1, wd[1][:, :n]),
                             (RW_NB0, nbd[0][:, :n]), (RW_NB1, nbd[1][:, :n]), (RW_KT0, ktd[0][:, :n]),
                             (RW_KT1, ktd[1][:, :n]), (RW_G, gg[:, :n]), (RW_BV, bv[:, :n])):
                k.dma(RW[idx, :, t0:t0 + n], src, eng=("act" if idx % 2 else "pool"), wk=(idx, ti_))
    outer = k.scope(); outer.__enter__()
    ysum = k.sb([64, Tx]); k.memset("pool", ysum[:], 0.0)
    with k.scope():
        SEG = 256
        ident = k.sb([128, 128]); k.memset("dve", ident[:], 1.0)
        k.aselect(ident[:], [[-1, 128]], ALU.is_equal, 0, 1)
        ST = [k.sb([64, 64]) for _ in range(2)]; T1 = [k.sb([64, 64]) for _ in range(2)]
        for d in range(2):
            k.memset("dve", ST[d][:], 0.0)
        segs = [dir_tiles(LCx, LLx, SEG, d) for d in range(2)]
        nseg = len(segs[0])
        sv = [[k.sb([64, 5, SEG]) for _ in range(2)] for d in range(2)]
        vtm = [[k.sb([128, 2, 64]) for _ in range(2)] for d in range(2)]
        Z = [[k.sb([64, 8, 64]) for _ in range(2)] for d in range(2)]
        p_sa = [k.ps([64, 64]) for d in range(2)]
        p_y = [k.ps([64, SEG]) for d in range(2)]
        p_vb = [[k.ps([64, 8, 64]) for _ in range(2)] for d in range(2)]
        ye = [k.sb([64, SEG]) for d in range(2)]

        def load(d, si):
            t0, n, c = segs[d][si]
            b = si % 2
            for q, idx in enumerate((RW_R, RW_KK, RW_W0 + d, RW_NB0 + d, RW_KT0 + d)):
                k.dma(sv[d][b][:, q, :], RW[idx, :, t0:t0 + n], eng=("sp", "act", "pool")[q % 3], wk=q)
            k.dma(vtm[d][b][:], Vtm[t0:t0 + n, :].rearrange("(a s) c -> s a c", s=128), eng="sp")

        for d in range(2):
            load(d, 0)
        for si in range(nseg):
            for d in range(2):
                if si + 1 < nseg:
                    load(d, si + 1)
            b = si % 2
            for g8 in range(SEG // 8 + 1):
                for d in range(2):
                    if g8 < SEG // 8:
                        vb = p_vb[d][g8 % 2]; zz = Z[d][g8 % 2]
                        cols = []
                        for jj in range(8):
                            i = g8 * 8 + jj
                            c = i if d == 0 else SEG - 1 - i
                            cols.append(c)
                            k.mm(vb[:, jj, :], ident[:, c % 128:c % 128 + 1].to_broadcast([128, 64]), vtm[d][b][:, c // 128, :])
                        for jj in range(8):
                            c = cols[jj]
                            k.act(zz[:, jj, :], vb[:, jj, :], AF.Copy, scale=sv[d][b][:, 4, c:c + 1], wk=jj,
                                  rk={sv[d][b].name: 4})
                for jj in range(8):
                    for d in range(2):
                        gp = g8 - 1
                        if gp < 0:
                            continue
                        i = gp * 8 + jj
                        c = i if d == 0 else SEG - 1 - i
                        s_ = sv[d][b]; zz = Z[d][gp % 2]
                        k.mm(p_sa[d][:], s_[:, 1, c:c + 1].to_broadcast([64, 64]), ST[d][:], rk={s_.name: 1})
                        k.stt(T1[d][:], p_sa[d][:], s_[:, 3, c:c + 1], zz[:, jj, :], ALU.mult, ALU.add,
                              rk={s_.name: 3, zz.name: jj})
                        k.stt(ST[d][:], ST[d][:], s_[:, 2, c:c + 1], T1[d][:], ALU.mult, ALU.add, rk={s_.name: 2})
                        k.mm(p_y[d][:, c:c + 1], ST[d][:], s_[:, 0, c:c + 1], rk={s_.name: 0})
            for d in range(2):
                t0, n, c = segs[d][si]
                k.copy("act", ye[d][:], p_y[d][:])
                k.tt("pool", ysum[:, t0:t0 + n], ysum[:, t0:t0 + n], ye[d][:], ALU.add, wk=t0, rk={ysum.name: t0})
    with k.scope():
        rv = k.sb([64, 16]); k.dma(rv[:], I["rwv"])
        o64 = k.sb([64, 64]); k.memset("dve", o64[:], 1.0 / 64.0)
        N = 512
        yc = k.sb([64, N]); sq = k.sb([64, N]); gt = [k.sb([64, N]) for _ in range(2)]; bt = [k.sb([64, N]) for _ in range(2)]
        pm = k.ps([64, N]); pvv = k.ps([64, N])
        for ti_, (t0, n, c) in enumerate(seq_tiles(LCx, LLx, N)):
            b = ti_ % 2
            k.dma(gt[b][:, :n], RW[RW_G, :, t0:t0 + n], eng="sp")
            k.dma(bt[b][:, :n], RW[RW_BV, :, t0:t0 + n], eng="act")
            k.mm(pm[:, :n], o64[:], ysum[:, t0:t0 + n], rk={ysum.name: None})
            k.tt("dve", yc[:, :n], ysum[:, t0:t0 + n], pm[:, :n], ALU.subtract, rk={ysum.name: None})
            k.act(sq[:, :n], yc[:, :n], AF.Square)
            k.mm(pvv[:, :n], o64[:], sq[:, :n])
            k.ts("dve", sq[:, :n], pvv[:, :n], 64e-5, None, ALU.add)
            k.act(sq[:, :n], sq[:, :n], AF.Sqrt)
            k.recip(sq[:, :n], sq[:, :n])
            k.tt("dve", yc[:, :n], yc[:, :n], sq[:, :n], ALU.mult)
            k.ts("dve", yc[:, :n], yc[:, :n], rv[:, 10:11], rv[:, 11:12], ALU.mult, ALU.add)
            k.tt("pool", yc[:, :n], yc[:, :n], bt[b][:, :n], ALU.add)
            k.tt("dve", yc[:, :n], yc[:, :n], gt[b][:, :n], ALU.mult)
            k.dma(O["yB"][:, t0:t0 + n], yc[:, :n], eng="pool")
    outer.__exit__(None, None, None)


def build_B(tiles):
    NL = sum(t[1] for t in tiles)
    nc = bass.Bass("TRN2", target_bir_lowering=False)
    k = KB(nc)
    I, O = _mk({"xT": [128, KC, NL], "brT": [128, KC, NL], "ogT": [128, 4, NL], "gT": [128, 64, NL],
                "wbr": [16, 128, KC, 128], "wout": [16, 128, KC, 128], "vecB": [128, KC, 8], "hgn": [128, 1],
                "rw": [128, KC, 64], "rb": [128, 64]},
               {"x1T": [128, KC, NL], "h2T": [128, KC, NL], "cw": [NL, 64]}, nc)
    vec = k.sb([128, KC, 8]); k.dma(vec[:], I["vecB"])
    hgn = k.sb([128, 1]); k.dma(hgn[:], I["hgn"], eng="act")
    rw = k.sb([128, KC, 64]); k.dma(rw[:], I["rw"], eng="act")
    rb = k.sb([128, 64]); k.dma(rb[:], I["rb"], eng="pool")
    g2 = k.sb([128, KC, 2])
    for s in range(2):
        k.ts("dve", g2[:, :, s], vec[:, :, 4 + 2 * s], 1.0, None, ALU.add, wk=s)
        k.tt("dve", g2[:, :, s], g2[:, :, s], vec[:, :, 2], ALU.mult, wk=s, rk={g2.name: s})
    onesD = k.sb([128, 128]); k.memset("dve", onesD[:], 1.0 / D)
    ones128 = k.sb([128, 128]); k.memset("dve", ones128[:], 1.0 / 128.0)
    N = 256
    xt = k.sb([128, KC, N]); br = k.sb([128, KC, N]); og = k.sb([128, 4, N]); mg = k.sb([128, KC, N])
    x1 = k.sb([128, KC, N]); sq = k.sb([128, KC, N])
    gt = [k.sb([128, 4, N]) for _ in range(2)]
    wb = [k.sb([128, KC, 128]) for _ in range(3)]
    tmp = [k.sb([128, N]) for _ in range(4)]
    rstd = k.sb([128, N])
    pk = [k.ps([128, N]) for _ in range(4)]
    pn = k.ps([128, N]); po = [k.ps([128, N]) for _ in range(2)]; pr = k.ps([128, 64])
    wi = 0
    for (c0, n, s) in tiles:
        k.dma(xt[:, :, :n], I["xT"][:, :, c0:c0 + n], eng="sp")
        k.dma(br[:, :, :n], I["brT"][:, :, c0:c0 + n], eng="act")
        k.dma(og[:, :, :n], I["ogT"][:, :, c0:c0 + n], eng="pool")
        k.act(sq[:, 0:4, :n], br[:, 8:12, :n], AF.Square)
        k.act(og[:, :, :n], og[:, :, :n], AF.Silu)
        for h in range(4):
            k.mm(pn[:, :n], ones128[:], sq[:, h, :n])
            k.ts("dve", rstd[:, :n], pn[:, :n], 1e-6, None, ALU.add)
            k.act(rstd[:, :n], rstd[:, :n], AF.Sqrt)
            k.recip(rstd[:, :n], rstd[:, :n])
            k.stt(br[:, 8 + h, :n], br[:, 8 + h, :n], hgn[:, 0:1], rstd[:, :n], ALU.mult, ALU.mult)
            k.tt("pool", br[:, 8 + h, :n], br[:, 8 + h, :n], og[:, h, :n], ALU.mult)
        gv = I["gT"].rearrange("p (k d) t -> p k d t", k=4)
        for dt in range(16):
            w = wb[wi % 3]; wi += 1
            k.dma(w[:], I["wbr"][dt], eng="sp")
            g = gt[dt % 2]
            k.dma(g[:, :, :n], gv[:, :, dt, c0:c0 + n], eng="act")
            for kb_i in range(4):
                for c in range(4):
                    k.mm(pk[kb_i][:, :n], w[:, kb_i * 4 + c, :], br[:, kb_i * 4 + c, :n], start=(c == 0), stop=(c == 3))
            k.tt("dve", mg[:, dt, :n], pk[0][:, :n], g[:, 0, :n], ALU.mult, wk=dt)
            for kb_i in range(1, 4):
                k.tt("dve", tmp[kb_i][:, :n], pk[kb_i][:, :n], g[:, kb_i, :n], ALU.mult)
                k.tt("pool", mg[:, dt, :n], mg[:, dt, :n], tmp[kb_i][:, :n], ALU.add, wk=dt, rk={mg.name: dt})
        for dt in range(16):
            w = wb[wi % 3]; wi += 1
            k.dma(w[:], I["wout"][dt], eng="sp")
            p = po[dt % 2]
            for kc in range(KC):
                k.mm(p[:, :n], w[:, kc, :], mg[:, kc, :n], start=(kc == 0), stop=(kc == KC - 1))
            k.stt(x1[:, dt, :n], p[:, :n], vec[:, dt, s:s + 1], xt[:, dt, :n], ALU.mult, ALU.add, wk=dt)
        k.dma(O["x1T"][:, :, c0:c0 + n], x1[:, :, :n], eng="pool")
        k.act(sq[:, :, :n], x1[:, :, :n], AF.Square)
        for kc in range(KC):
            k.mm(pn[:, :n], onesD[:], sq[:, kc, :n], start=(kc == 0), stop=(kc == KC - 1))
        k.ts("dve", rstd[:, :n], pn[:, :n], 1e-6, None, ALU.add)
        k.act(rstd[:, :n], rstd[:, :n], AF.Sqrt)
        k.recip(rstd[:, :n], rstd[:, :n])
        h2 = sq
        k.tt("dve", h2[:, :, :n], x1[:, :, :n], rstd[:, :n].unsqueeze(1).to_broadcast([128, KC, n]), ALU.mult)
        k.tt("pool", h2[:, :, :n], h2[:, :, :n], g2[:, :, s:s + 1].to_broadcast([128, KC, n]), ALU.mult)
        k.tt("dve", h2[:, :, :n], h2[:, :, :n], vec[:, :, 3 + 2 * s:4 + 2 * s].to_broadcast([128, KC, n]), ALU.add)
        k.dma(O["h2T"][:, :, c0:c0 + n], h2[:, :, :n], eng="act")
        for s0 in range(0, n, 128):
            m = min(128, n - s0)
            for kc in range(KC):
                k.mm(pr[:m, :], h2[:, kc, s0:s0 + m], rw[:, kc, :], start=(kc == 0), stop=(kc == KC - 1))
            sc = k_router(k, pr, rb, m)
            k.dma(O["cw"][c0 + s0:c0 + s0 + m, :], sc, eng="sp")
    k.emit(); k.close()
    return nc


def k_router(k, pr, rb, m):
    if not hasattr(k, "_rt"):
        k._rt = {n_: k.sb([128, 64]) for n_ in ("sc", "sel", "eq", "sel2", "ge", "cw")}
        k._rt.update({n_: k.sb([128, 8]) for n_ in ("m1", "m2", "gs", "gsel")})
        k._rt.update({n_: k.sb([128, 1]) for n_ in ("gmax", "ws")})
    t = k._rt
    v3 = lambda a: a[:m, :].rearrange("p (g e) -> p g e", e=8)
    b3 = lambda a: a[:m, :].unsqueeze(2).to_broadcast([m, 8, 8])
    k.act(t["sc"][:m, :], pr[:m, :], AF.Sigmoid)
    k.tt("dve", t["sel"][:m, :], t["sc"][:m, :], rb[:m, :], ALU.add)
    k.op("dve", lambda e: e.tensor_reduce(t["m1"][:m, :], v3(t["sel"]), AX.X, ALU.max), R=[t["sel"]], W=[t["m1"]])
    k.tt("dve", v3(t["eq"]), v3(t["sel"]), b3(t["m1"]), ALU.is_equal)
    k.stt(t["sel2"][:m, :], t["eq"][:m, :], -1e9, t["sel"][:m, :], ALU.mult, ALU.add)
    k.op("dve", lambda e: e.tensor_reduce(t["m2"][:m, :], v3(t["sel2"]), AX.X, ALU.max), R=[t["sel2"]], W=[t["m2"]])
    k.tt("dve", t["gs"][:m, :], t["m1"][:m, :], t["m2"][:m, :], ALU.add)
    k.op("dve", lambda e: e.tensor_reduce(t["gmax"][:m, :], t["gs"][:m, :], AX.X, ALU.max), R=[t["gs"]], W=[t["gmax"]])
    k.ts("dve", t["gsel"][:m, :], t["gs"][:m, :], t["gmax"][:m, 0:1], None, ALU.is_equal)
    k.tt("dve", v3(t["ge"]), v3(t["sel"]), b3(t["m2"]), ALU.is_ge)
    k.tt("dve", v3(t["ge"]), v3(t["ge"]), b3(t["gsel"]), ALU.mult)
    k.tt("dve", t["cw"][:m, :], t["ge"][:m, :], t["sc"][:m, :], ALU.mult)
    k.op("dve", lambda e: e.tensor_reduce(t["ws"][:m, :], t["cw"][:m, :], AX.X, ALU.add), R=[t["cw"]], W=[t["ws"]])
    k.recip(t["ws"][:m, :], t["ws"][:m, :])
    k.ts("dve", t["cw"][:m, :], t["cw"][:m, :], t["ws"][:m, 0:1], None, ALU.mult)
    return t["cw"][:m, :]


def build_C(Tx, NE=8, mm_dt=None):
    nc = bass.Bass("TRN2", target_bir_lowering=False)
    k = KB(nc)
    I, O = _mk({"h2T": [128, KC, Tx], "cwT": [NE, Tx], "wg": [NE, 6, 128, KC, 128], "wu": [NE, 6, 128, KC, 128],
                "wd": [NE, 16, 128, 6, 128]}, {"yT": [128, KC, Tx]}, nc)
    N = 512
    h2 = [k.sb([128, KC, N]) for _ in range(2)]
    cwb = [k.sb([128, NE, N]) for _ in range(2)]
    yacc = k.sb([128, KC, N]); act = k.sb([128, 6, N]); sg = [k.sb([128, N]) for _ in range(2)]
    wgb = [k.sb([128, KC, 128]) for _ in range(2)]; wub = [k.sb([128, KC, 128]) for _ in range(2)]
    wdb = [k.sb([128, 6, 128]) for _ in range(3)]
    pg = [k.ps([128, N]) for _ in range(2)]; pu = [k.ps([128, N]) for _ in range(2)]; py = [k.ps([128, N]) for _ in range(3)]
    cast = (lambda a: a.bitcast(mm_dt)) if mm_dt is not None else (lambda a: a)
    wi = 0; di = 0
    tiles = [(t0, min(N, Tx - t0)) for t0 in range(0, Tx, N)]
    for ti, (t0, n) in enumerate(tiles):
        b = ti % 2
        k.dma(h2[b][:, 0:8, :n], I["h2T"][:, 0:8, t0:t0 + n], eng="sp")
        k.dma(h2[b][:, 8:16, :n], I["h2T"][:, 8:16, t0:t0 + n], eng="act")
        k.dma(cwb[b][:, :, :n], I["cwT"][:, t0:t0 + n].partition_broadcast(128), eng="pool")
        for e in range(NE):
            for fc in range(6):
                wg_ = wgb[wi % 2]; wu_ = wub[wi % 2]; p1 = pg[wi % 2]; p2 = pu[wi % 2]; s_ = sg[wi % 2]; wi += 1
                k.dma(wg_[:], I["wg"][e, fc], eng="sp")
                k.dma(wu_[:], I["wu"][e, fc], eng="act")
                for kc in range(KC):
                    k.mm(p1[:, :n], cast(wg_[:, kc, :]), cast(h2[b][:, kc, :n]), start=(kc == 0), stop=(kc == KC - 1))
                for kc in range(KC):
                    k.mm(p2[:, :n], cast(wu_[:, kc, :]), cast(h2[b][:, kc, :n]), start=(kc == 0), stop=(kc == KC - 1))
                k.act(s_[:, :n], p1[:, :n], AF.Silu)
                k.tt("dve", s_[:, :n], s_[:, :n], p2[:, :n], ALU.mult)
                k.tt("pool", act[:, fc, :n], s_[:, :n], cwb[b][:, e, :n], ALU.mult, wk=fc)
            for dt in range(16):
                wd_ = wdb[di % 3]; p3 = py[di % 3]; di += 1
                k.dma(wd_[:], I["wd"][e, dt], eng="pool")
                for fc in range(6):
                    k.mm(p3[:, :n], cast(wd_[:, fc, :]), cast(act[:, fc, :n]), start=(fc == 0), stop=(fc == 5))
                if e == 0:
                    k.copy("act", yacc[:, dt, :n], p3[:, :n], wk=dt)
                else:
                    k.tt("dve", yacc[:, dt, :n], yacc[:, dt, :n], p3[:, :n], ALU.add, wk=dt, rk={yacc.name: dt})
        k.dma(O["yT"][:, 0:8, t0:t0 + n], yacc[:, 0:8, :n], eng="sp")
        k.dma(O["yT"][:, 8:16, t0:t0 + n], yacc[:, 8:16, :n], eng="act")
    k.emit(); k.close()
    return nc


def build_D(tiles, final):
    NL = sum(t[1] for t in tiles)
    nc = bass.Bass("TRN2", target_bir_lowering=False)
    k = KB(nc)
    I, O = _mk({"yp": [NCORE, 128, KC, NL], "x1T": [128, KC, NL], "vecE": [128, KC, 3]},
               {"x2T": [128, KC, NL]}, nc)
    vec = k.sb([128, KC, 3]); k.dma(vec[:], I["vecE"])
    onesD = k.sb([128, 128]); k.memset("dve", onesD[:], 1.0 / D)
    N = 512
    acc = k.sb([128, KC, N]); x1 = k.sb([128, KC, N]); sq = k.sb([128, KC, N]); rstd = k.sb([128, N])
    yb = [k.sb([128, KC, N]) for _ in range(2)]
    pn = k.ps([128, N])
    for (c0, n, s) in tiles:
        k.dma(x1[:, :, :n], I["x1T"][:, :, c0:c0 + n], eng="pool")
        for j in range(NCORE):
            b = j % 2
            k.dma(yb[b][:, :, :n], I["yp"][j, :, :, c0:c0 + n], eng=("sp" if b == 0 else "act"))
            if j == 0:
                k.copy("dve", acc[:, :, :n], yb[b][:, :, :n])
            else:
                k.tt("dve" if j % 2 else "pool", acc[:, :, :n], acc[:, :, :n], yb[b][:, :, :n], ALU.add)
        k.tt("dve", acc[:, :, :n], acc[:, :, :n], vec[:, :, s:s + 1].to_broadcast([128, KC, n]), ALU.mult)
        k.tt("pool", x1[:, :, :n], x1[:, :, :n], acc[:, :, :n], ALU.add)
        if final:
            k.act(sq[:, :, :n], x1[:, :, :n], AF.Square)
            for kc in range(KC):
                k.mm(pn[:, :n], onesD[:], sq[:, kc, :n], start=(kc == 0), stop=(kc == KC - 1))
            k.ts("dve", rstd[:, :n], pn[:, :n], 1e-6, None, ALU.add)
            k.act(rstd[:, :n], rstd[:, :n], AF.Sqrt)
            k.recip(rstd[:, :n], rstd[:, :n])
            k.tt("dve", x1[:, :, :n], x1[:, :, :n], rstd[:, :n].unsqueeze(1).to_broadcast([128, KC, n]), ALU.mult)
            k.tt("pool", x1[:, :, :n], x1[:, :, :n], vec[:, :, 2:3].to_broadcast([128, KC, n]), ALU.mult)
        k.dma(O["x2T"][:, :, c0:c0 + n], x1[:, :, :n], eng="sp")
    k.emit(); k.close()
    return nc


def rows_fm(a):
    R, Tn = a.shape
    return a.reshape(R // 128, 128, Tn).transpose(1, 0, 2)


def _run(nc, maps):
    return run_bass_kernel_spmd(nc, maps, core_ids=list(range(len(maps)))).results


def _tiles(nctx, nlat, n):
    out = [(0, nctx, 1)]
    t = 0
    while t < nlat:
        m_ = min(n, nlat - t)
        out.append((nctx + t, m_, 0)); t += m_
    return out


def forward(inp, dbg=None):
    inp = {k_: np.asarray(v) for k_, v in inp.items()}
    LCx = inp["ctx"].shape[1]; LLx = inp["x"].shape[1]; Tx = LCx + LLx
    nctx = LCx // NCORE; nlat = LLx // NCORE; NL = nctx + nlat
    m, x0T = run_M(inp)
    xfull = np.ascontiguousarray(np.concatenate([fm(inp["ctx"][0]), x0T], axis=2))
    idx = [np.concatenate([np.arange(nctx * j, nctx * (j + 1)), LCx + np.arange(nlat * j, nlat * (j + 1))])
           for j in range(NCORE)]
    tilesB = _tiles(nctx, nlat, 256); tilesD = _tiles(nctx, nlat, 512)
    ncB = build_B(tilesB); ncC = build_C(Tx)
    rw = np.ascontiguousarray(inp["router_w"].reshape(KC, 128, 64).transpose(1, 0, 2))
    rb = np.ascontiguousarray(np.broadcast_to(inp["router_b"][None, :], (128, 64)))
    for l in range(2):
        last = (l == 1)
        mm_ = m[l].reshape(2, 6, D)
        ncA = build_A(l, LCx, LLx)
        rA = _run(ncA, [a_host_inputs(inp, l, j, xfull, m) for j in range(NCORE)])
        brT = np.concatenate([np.concatenate([rA[j][nm] for j in range(NCORE)], 0) for nm in ("yA", "yB", "yC", "yD")], 0)
        ogT = np.concatenate([rA[j]["ogT"] for j in range(NCORE)], 0)
        gT = np.concatenate([rA[j]["gT"] for j in range(NCORE)], 0)
        if dbg is not None:
            dbg[f"brT{l}"] = brT; dbg[f"gT{l}"] = gT
        del rA
        brF = rows_fm(brT); ogF = rows_fm(ogT); gF = rows_fm(gT)
        wbr = np.ascontiguousarray(inp["w_branch"][l].reshape(16, 128, 16, 128).transpose(2, 1, 0, 3))
        wout = np.ascontiguousarray(inp["w_out"][l].reshape(16, 128, 16, 128).transpose(2, 1, 0, 3))
        vecB = np.ascontiguousarray(np.stack([fm(mm_[0, 2]), fm(mm_[1, 2]), fm(inp["norm_ffn_g"][l]), fm(mm_[0, 3]),
                                              fm(mm_[0, 4]), fm(mm_[1, 3]), fm(mm_[1, 4]), fm(mm_[1, 4])], axis=-1))
        hgn = np.ascontiguousarray(inp["hg_norm_g"][l][:, None])
        mapsB = [{"xT": np.ascontiguousarray(xfull[:, :, idx[j]]), "brT": np.ascontiguousarray(brF[:, :, idx[j]]),
                  "ogT": np.ascontiguousarray(ogF[:, :, idx[j]]), "gT": np.ascontiguousarray(gF[:, :, idx[j]]),
                  "wbr": wbr, "wout": wout, "vecB": vecB, "hgn": hgn, "rw": rw, "rb": rb} for j in range(NCORE)]
        del brF, ogF, gF, gT, brT, ogT
        rB = _run(ncB, mapsB)
        del mapsB
        h2full = np.empty((128, KC, Tx), np.float32); cwfull = np.empty((Tx, 64), np.float32)
        for j in range(NCORE):
            h2full[:, :, idx[j]] = rB[j]["h2T"]; cwfull[idx[j], :] = rB[j]["cw"]
        if dbg is not None:
            dbg[f"h2_{l}"] = h2full.copy(); dbg[f"cw_{l}"] = cwfull.copy()
            dbg[f"x1_{l}"] = np.concatenate([rB[j]["x1T"] for j in range(NCORE)], 2)
        mapsC = []
        for j in range(NCORE):
            es = slice(8 * j, 8 * j + 8)
            wg = inp["moe_w_gate"][l, es].reshape(8, KC, 128, 6, 128).transpose(0, 3, 2, 1, 4)
            wu = inp["moe_w_up"][l, es].reshape(8, KC, 128, 6, 128).transpose(0, 3, 2, 1, 4)
            wd = inp["moe_w_down"][l, es].reshape(8, 6, 128, 16, 128).transpose(0, 3, 2, 1, 4)
            mapsC.append({"h2T": h2full, "cwT": np.ascontiguousarray(cwfull[:, es].T), "wg": np.ascontiguousarray(wg),
                          "wu": np.ascontiguousarray(wu), "wd": np.ascontiguousarray(wd)})
        rC = _run(ncC, mapsC)
        del mapsC
        vecE = np.ascontiguousarray(np.stack([fm(mm_[0, 5]), fm(mm_[1, 5]), fm(inp["final_norm_g"])], axis=-1))
        mapsD = [{"yp": np.ascontiguousarray(np.stack([rC[jj]["yT"][:, :, idx[j]] for jj in range(NCORE)], 0)),
                  "x1T": rB[j]["x1T"], "vecE": vecE} for j in range(NCORE)]
        del rC
        ncD = build_D(tilesD, last)
        rD = _run(ncD, mapsD)
        del mapsD
        for j in range(NCORE):
            xfull[:, :, idx[j]] = rD[j]["x2T"]
        if dbg is not None:
            dbg[f"x2_{l}"] = xfull.copy()
    out = xfull[:, :, LCx:].transpose(2, 1, 0).reshape(LLx, D)[None]
    return np.ascontiguousarray(out.astype(np.float32))


def kernel(**inputs):
    return forward(inputs)
```

```python
import numpy as np
import concourse.bass as bass
import concourse.mybir as mybir
from concourse.bass_utils import run_bass_kernel_spmd
from contextlib import ExitStack

F32 = mybir.dt.float32
BF16 = mybir.dt.bfloat16
I32 = mybir.dt.int32
U32 = mybir.dt.uint32
AF = mybir.ActivationFunctionType
ALU = mybir.AluOpType
AX = mybir.AxisListType

SEM_GEN = 20000
N_DMA_SEMS = 12


class KB:
    ENG = ("pe", "dve", "act", "pool", "sp")

    def __init__(self, nc):
        self.nc = nc
        self.es = ExitStack()
        self.q = {e: [] for e in self.ENG}
        self.lastw = {}
        self.readers = {}
        self.n_t = 0
        self.dma_count = {e: 0 for e in self.ENG}
        self.bar_pos = {e: 0 for e in self.ENG}
        self.bar_recs = []
        self.bar_seen = set()

    def sb(self, shape, dt=F32, name=None):
        self.n_t += 1
        return self.es.enter_context(self.nc.sbuf_tensor(name or f"sb{self.n_t}", list(shape), dt))

    def ps(self, shape, dt=F32, name=None):
        self.n_t += 1
        return self.es.enter_context(self.nc.psum_tensor(name or f"ps{self.n_t}", list(shape), dt))

    def scope(self):
        kb = self

        class _S:
            def __enter__(s_):
                s_.old = kb.es
                kb.es = ExitStack()
                return s_

            def __exit__(s_, *a):
                kb.es.close()
                kb.es = s_.old
                kb.barrier()
                return False
        return _S()

    def barrier(self):
        recs = []
        for e in self.ENG:
            last = None
            for r in self.q[e][self.bar_pos[e]:]:
                if r["dma"]:
                    recs.append(r)
                else:
                    last = r
            if last is not None:
                recs.append(last)
            self.bar_pos[e] = len(self.q[e])
        for r in recs:
            r["signal"] = True
        new_eng = {r["eng"] for r in recs if not r["dma"]}
        self.bar_recs = recs + [r for r in self.bar_recs if r["eng"] not in new_eng]
        self.bar_seen = set()

    def dram(self, name, shape, dt=F32, kind="Internal"):
        return self.nc.dram_tensor(name, list(shape), dt, kind=kind)

    def _res(self, x, key):
        if isinstance(x, tuple):
            x, key = x
        if isinstance(x, str):
            n = x
        elif hasattr(x, "tensor"):
            n = x.tensor.name
        else:
            n = x.name
        if isinstance(key, dict):
            key = key.get(n)
        return (n, key)

    def op(self, eng, fn, R=(), W=(), dma=False, rk=None, wk=None):
        rec = {"eng": eng, "fn": fn, "deps": [], "signal": False, "dma": dma, "tok": None}
        deps = []
        Rr = [self._res(r, rk) for r in R if r is not None and not isinstance(r, (int, float))]
        Ww = [self._res(w, wk) for w in W]
        for (t, k) in Rr:
            lw = self.lastw.get(t)
            if lw:
                if k is None:
                    deps.extend(lw.values())
                else:
                    if k in lw: deps.append(lw[k])
                    if None in lw: deps.append(lw[None])
        for (t, k) in Ww:
            lw = self.lastw.get(t)
            rd = self.readers.get(t)
            if lw:
                if k is None:
                    deps.extend(lw.values())
                else:
                    if k in lw: deps.append(lw[k])
                    if None in lw: deps.append(lw[None])
            if rd:
                if k is None:
                    for l in rd.values(): deps.extend(l)
                else:
                    if k in rd: deps.extend(rd[k])
                    if None in rd: deps.extend(rd[None])
        if self.bar_recs and eng not in self.bar_seen:
            self.bar_seen.add(eng)
            for d in self.bar_recs:
                if not (d["eng"] == eng and not d["dma"] and eng == "pe"):
                    rec["deps"].append(d)
        seen = set(id(d) for d in rec["deps"])
        for d in deps:
            if d is rec or id(d) in seen:
                continue
            seen.add(id(d))
            if d["eng"] == "pe" and eng == "pe" and not d["dma"] and not dma:
                continue
            d["signal"] = True
            rec["deps"].append(d)
        for (t, k) in Rr:
            self.readers.setdefault(t, {}).setdefault(k, []).append(rec)
        for (t, k) in Ww:
            if k is None:
                self.lastw[t] = {None: rec}
                self.readers[t] = {}
            else:
                self.lastw.setdefault(t, {})[k] = rec
                rd = self.readers.setdefault(t, {})
                rd[k] = []
        if dma:
            rec["signal"] = True
        self.q[eng].append(rec)
        return rec

    def emit(self):
        nc = self.nc
        sems = {}
        for e in self.ENG:
            n_sig = sum(1 for r in self.q[e] if r["signal"] and not r["dma"])
            n_gen = max(1, -(-n_sig // SEM_GEN))
            gens = [self.es.enter_context(nc.semaphore(f"s_{e}_{g}")) for g in range(n_gen)]
            dsem = [self.es.enter_context(nc.semaphore(f"d_{e}_{g}")) for g in range(N_DMA_SEMS)] \
                if any(r["dma"] for r in self.q[e]) else []
            c = 0
            dc = 0
            for r in self.q[e]:
                if r["dma"]:
                    slot = dc % N_DMA_SEMS
                    r["tok"] = (dsem[slot], 16 * (dc // N_DMA_SEMS + 1), 16)
                    r["prev"] = (dsem[slot], 16 * (dc // N_DMA_SEMS))
                    dc += 1
                elif r["signal"]:
                    g = c // SEM_GEN
                    r["tok"] = (gens[g], c % SEM_GEN + 1, 1)
                    c += 1
        engobj = {"pe": nc.tensor, "dve": nc.vector, "act": nc.scalar, "pool": nc.gpsimd, "sp": nc.sync}
        block = self.es.enter_context(nc.Block())

        def run(e, eo):
            waited = {}
            for r in self.q[e]:
                need = {}
                for d in r["deps"]:
                    s, v, _ = d["tok"]
                    if waited.get(id(s), (None, 0))[1] < v:
                        if id(s) not in need or need[id(s)][1] < v:
                            need[id(s)] = (s, v)
                if r["dma"] and r["prev"][1] > 0:
                    s, v = r["prev"]
                    if waited.get(id(s), (None, 0))[1] < v and (id(s) not in need or need[id(s)][1] < v):
                        need[id(s)] = (s, v)
                for s, v in need.values():
                    eo.wait_ge(s, v)
                    waited[id(s)] = (s, v)
                inst = r["fn"](eo)
                if r["signal"]:
                    s, v, inc = r["tok"]
                    inst.then_inc(s, inc)
            for r in self.q[e]:
                if r["dma"]:
                    s, v, _ = r["tok"]
                    if waited.get(id(s), (None, 0))[1] < v:
                        eo.wait_ge(s, v)
                        waited[id(s)] = (s, v)

        if self.q["sp"]:
            @block.sync
            def _(eo):
                run("sp", eo)
        if self.q["act"]:
            @block.scalar
            def _(eo):
                run("act", eo)
        if self.q["pool"]:
            @block.gpsimd
            def _(eo):
                run("pool", eo)
        if self.q["dve"]:
            @block.vector
            def _(eo):
                run("dve", eo)
        if self.q["pe"]:
            @block.tensor
            def _(eo):
                run("pe", eo)

    def close(self):
        self.es.close()

    def dma(self, out, in_, eng="sp", rk=None, wk=None, **kw):
        return self.op(eng, lambda e: e.dma_start(out=out, in_=in_, **kw), R=[in_], W=[out], dma=True, rk=rk, wk=wk)

    def mm(self, out, lhsT, rhs, start=True, stop=True, rk=None, wk=None, R=(), **kw):
        return self.op("pe", lambda e: e.matmul(out, lhsT, rhs, start=start, stop=stop, **kw),
                       R=[lhsT, rhs] + list(R), W=[out], rk=rk, wk=wk)

    def tr(self, out, in_, ident, rk=None, wk=None):
        return self.op("pe", lambda e: e.transpose(out, in_, ident), R=[in_, ident], W=[out], rk=rk, wk=wk)

    def act(self, out, in_, func, eng="act", rk=None, wk=None, R=(), **kw):
        extra = [v for v in (kw.get("bias"), kw.get("scale"), kw.get("accum_out")) if hasattr(v, "tensor")]
        Wl = [out] + ([kw["accum_out"]] if kw.get("accum_out") is not None else [])
        return self.op(eng, lambda e: e.activation(out=out, in_=in_, func=func, **kw),
                       R=[in_] + extra + list(R), W=Wl, rk=rk, wk=wk)

    def ts(self, eng, out, in0, s1, s2, op0, op1=None, rk=None, wk=None, R=(), **kw):
        extra = [v for v in (s1, s2) if hasattr(v, "tensor")]
        if op1 is None:
            return self.op(eng, lambda e: e.tensor_scalar(out, in0, s1, None, op0, **kw),
                           R=[in0] + extra + list(R), W=[out], rk=rk, wk=wk)
        return self.op(eng, lambda e: e.tensor_scalar(out, in0, s1, s2, op0, op1, **kw),
                       R=[in0] + extra + list(R), W=[out], rk=rk, wk=wk)

    def tt(self, eng, out, in0, in1, op, rk=None, wk=None, R=()):
        return self.op(eng, lambda e: e.tensor_tensor(out, in0, in1, op), R=[in0, in1] + list(R), W=[out], rk=rk, wk=wk)

    def stt(self, out, in0, scalar, in1, op0, op1, eng="dve", rk=None, wk=None, R=()):
        extra = [scalar] if hasattr(scalar, "tensor") else []
        return self.op(eng, lambda e: e.scalar_tensor_tensor(out, in0, scalar, in1, op0, op1),
                       R=[in0, in1] + extra + list(R), W=[out], rk=rk, wk=wk)

    def copy(self, eng, out, in_, rk=None, wk=None):
        if eng == "act":
            return self.op(eng, lambda e: e.copy(out, in_), R=[in_], W=[out], rk=rk, wk=wk)
        return self.op(eng, lambda e: e.tensor_copy(out, in_), R=[in_], W=[out], rk=rk, wk=wk)

    def memset(self, eng, ap, val, wk=None):
        return self.op(eng, lambda e: e.memset(ap, val), W=[ap], wk=wk)

    def scan(self, out, d0, d1, init, op0, op1, rk=None, wk=None):
        extra = [init] if hasattr(init, "tensor") else []
        return self.op("dve", lambda e: e.tensor_tensor_scan(out, d0, d1, init, op0, op1),
                       R=[d0, d1] + extra, W=[out], rk=rk, wk=wk)

    def recip(self, out, in_, eng="dve", rk=None, wk=None):
        return self.op(eng, lambda e: e.reciprocal(out, in_), R=[in_], W=[out], rk=rk, wk=wk)

    def aselect(self, t, pattern, cmp, base, cm, fill=0.0):
        return self.op("pool", lambda e: e.affine_select(t, t, pattern, cmp, fill, base=base, channel_multiplier=cm),
                       R=[t], W=[t])

    def iota(self, t, pattern, base, cm):
        return self.op("pool", lambda e: e.iota(t, pattern, base=base, channel_multiplier=cm), W=[t])

    def gen(self, eng, fn, R=(), W=(), rk=None, wk=None):
        return self.op(eng, fn, R=R, W=W, rk=rk, wk=wk)


D = 2048; KC = 16; NCORE = 8
LC = 256; LL = 8192; T = LC + LL
BR = 512
TWO_PI_SAFE = 6.283185


def _mk(name_shapes_in, name_shapes_out, nc):
    ins = {n: nc.dram_tensor(n, list(s), F32, kind="ExternalInput").ap() for n, s in name_shapes_in.items()}
    outs = {n: nc.dram_tensor(n, list(s), F32, kind="ExternalOutput").ap() for n, s in name_shapes_out.items()}
    return ins, outs


def frac_sin(k, out, y, tmp_i, tmp_f, eng="dve", wk=None):
    k.copy(eng, tmp_i, y)
    k.copy(eng, tmp_f, tmp_i)
    k.tt(eng, tmp_f, y, tmp_f, ALU.subtract)
    k.act(out, tmp_f, AF.Sin, scale=TWO_PI_SAFE)


def build_M(NTK=1024):
    nc = bass.Bass("TRN2", target_bir_lowering=False)
    k = KB(nc)
    NT = 12
    I, O = _mk({"wm": [2, 128, KC, 1536], "bm": [2, 128, NT], "cc": [128, KC, 2],
                "xT": [128, KC, NTK], "tokpos": [2, NTK]},
               {"mT": [2, 128, NT, 2], "x0T": [128, KC, NTK]}, nc)
    cc = k.sb([128, KC, 2]); sc = k.sb([128, KC, 2]); bm = k.sb([128, 2, NT])
    k.dma(cc[:], I["cc"]); k.act(sc[:], cc[:], AF.Silu)
    for l in range(2):
        k.dma(bm[:, l, :], I["bm"][l], eng="act", wk=l)
    wmb = k.sb([128, KC, 1536])
    mo = k.sb([128, 2, NT, 2])
    pss = [k.ps([128, 2]) for _ in range(2)]
    for l in range(2):
        for g in range(4):
            k.dma(wmb[:, 4 * g:4 * g + 4, :], I["wm"][l, :, 4 * g:4 * g + 4, :], eng=("sp" if g % 2 == 0 else "act"), wk=g)
        for t in range(NT):
            ps = pss[t % 2]
            for kc in range(KC):
                k.mm(ps[:], wmb[:, kc, t * 128:(t + 1) * 128], sc[:, kc, :], start=(kc == 0), stop=(kc == KC - 1),
                     rk={wmb.name: kc // 4})
            k.ts("dve", mo[:, l, t, :], ps[:], bm[:, l, t:t + 1], None, ALU.add, wk=(l, t), rk={bm.name: l})
        k.dma(O["mT"][l], mo[:, l], eng="pool", rk=None)
    fi_i = k.sb([128, KC], I32); fi = k.sb([128, KC]); om = k.sb([128, KC]); ph = k.sb([128, KC])
    k.iota(fi_i[:], [[0, 4], [128, 4]], 0, 1)
    k.copy("dve", fi[:], fi_i[:])
    k.act(om[:], fi[:], AF.Exp, scale=-float(np.log(10000.0)) / 512.0)
    k.ts("dve", om[:], om[:], float(1.0 / (2.0 * np.pi)), None, ALU.mult)
    k.memset("dve", ph[:], 0.0)
    k.memset("dve", ph[:, 4:8], 0.25); k.memset("dve", ph[:, 12:16], 0.25)
    posb = k.sb([128, 2, NTK])
    k.dma(posb[:], I["tokpos"].partition_broadcast(128))
    xs = [k.sb([128, NTK]) for _ in range(2)]; ys = [k.sb([128, NTK]) for _ in range(2)]
    yi = [k.sb([128, NTK], I32) for _ in range(2)]; yf = [k.sb([128, NTK]) for _ in range(2)]
    for kc in range(KC):
        b = kc % 2
        k.dma(xs[b][:], I["xT"][:, kc, :], eng=("sp" if b == 0 else "act"))
        k.ts("dve", ys[b][:], posb[:, 0 if kc < 8 else 1, :], om[:, kc:kc + 1], ph[:, kc:kc + 1], ALU.mult, ALU.add)
        frac_sin(k, ys[b][:], ys[b][:], yi[b][:], yf[b][:])
        k.tt("dve", xs[b][:], xs[b][:], ys[b][:], ALU.add)
        k.dma(O["x0T"][:, kc, :], xs[b][:], eng="pool")
    k.emit(); k.close()
    return nc


def fm(a):
    a = np.asarray(a)
    lead = a.shape[:-1]
    b = a.reshape(lead + (KC, 128))
    nd = b.ndim
    perm = (nd - 1, nd - 2) + tuple(range(nd - 2))
    return np.ascontiguousarray(b.transpose(perm))


def run_M(inp):
    LLx = inp["x"].shape[1]
    NTK = LLx // NCORE
    ncm = build_M(NTK)
    w_mod = inp["w_mod"]; b_mod = inp["b_mod"]
    cc = np.ascontiguousarray(np.stack([fm(inp["c"][0]), fm(inp["c_ctx"])], axis=-1))
    maps = []
    for j in range(NCORE):
        wm = w_mod[:, :, j * 1536:(j + 1) * 1536].reshape(2, KC, 128, 1536).transpose(0, 2, 1, 3)
        bm = b_mod[:, j * 1536:(j + 1) * 1536].reshape(2, 12, 128).transpose(0, 2, 1)
        xT = fm(inp["x"][0, j * NTK:(j + 1) * NTK, :])
        t = np.arange(j * NTK, (j + 1) * NTK)
        tokpos = np.stack([(t // 64), (t % 64)]).astype(np.float32)
        maps.append({"wm": np.ascontiguousarray(wm), "bm": np.ascontiguousarray(bm), "cc": cc,
                     "xT": xT, "tokpos": tokpos})
    res = run_bass_kernel_spmd(ncm, maps, core_ids=list(range(NCORE))).results
    m = np.zeros((2, 2, 6 * D), np.float32)
    for j in range(NCORE):
        mT = res[j]["mT"]
        m[:, :, j * 1536:(j + 1) * 1536] = mT.transpose(0, 3, 2, 1).reshape(2, 2, 1536)
    x0T = np.concatenate([res[j]["x0T"] for j in range(NCORE)], axis=2)
    return m, x0T


def a_colspec(j):
    g = j // 2; h = j // 2; d = j % 2
    tiles = [
        [(128 * g, 128)],
        [(512 + 64 * j, 64), (1024 + 64 * j, 64)],
        [(1536 + 64 * j, 64), (5120 + 64 * j, 64)],
        [(2048, 128)], [(2176, 128)], [(2304, 128)], [(2432, 128)],
        [(2560 + 128 * h, 128)],
        [(3072 + 128 * h, 128)],
        [(4096 + 64 * j, 64), (4096 + 64 * j, 64)],
        [(4608 + 64 * j, 64), (5632 + 64 * j, 64)],
        [(3584 + 128 * h, 128)],
    ] + [[(6144 + 1024 * j + 128 * i, 128)] for i in range(8)]
    return tiles

NCT = 20
HG_DEBUG = None
NG0 = 12
P_A, P_R, P_K, P_V, P_LX, P_X, P_Q, P_F, P_LG = 0, 128, 192, 256, 320, 384, 896, 1024, 1152
P_F2 = 1216
P_ROWS = 1344


def seq_tiles(LCx, LLx, n):
    out = []
    for (s0, L, c) in ((0, LCx, 1), (LCx, LLx, 0)):
        t = 0
        while t < L:
            m = min(n, L - t)
            out.append((s0 + t, m, c))
            t += m
    return out


def a_stage_proj(k, I, O, P, Vi, LCx, LLx):
    with k.scope():
        vec = k.sb([128, KC, 5]); k.dma(vec[:], I["vecD"])
        g1 = k.sb([128, KC, 2]); shf = k.sb([128, KC, 2])
        for s in range(2):
            k.ts("dve", g1[:, :, s], vec[:, :, 2 + 2 * s], 1.0, None, ALU.add, wk=s)
            k.tt("dve", g1[:, :, s], g1[:, :, s], vec[:, :, 0], ALU.mult, wk=s, rk={g1.name: s})
            k.copy("dve", shf[:, :, s], vec[:, :, 1 + 2 * s], wk=s)
        ones = k.sb([128, 128]); k.memset("dve", ones[:], 1.0 / D)
        xb = [k.sb([128, KC, 512]) for _ in range(2)]
        sq = k.sb([128, KC, 512])
        hT = [k.sb([128, KC, 512]) for _ in range(2)]
        rstd = k.sb([128, 512])
        wbuf = [k.sb([128, KC, 128]) for _ in range(3)]
        stg = [k.sb([128, 512]) for _ in range(3)]
        pn = k.ps([128, 512])
        pp = [k.ps([128, 512]) for _ in range(3)]
        wi = 0; si = 0
        for ti, (t0, n, s) in enumerate(seq_tiles(LCx, LLx, 512)):
            b = ti % 2
            k.dma(xb[b][:, 0:8, :n], I["xT"][:, 0:8, t0:t0 + n], eng="sp", wk=0)
            k.dma(xb[b][:, 8:16, :n], I["xT"][:, 8:16, t0:t0 + n], eng="act", wk=1)
            k.act(sq[:, :, :n], xb[b][:, :, :n], AF.Square)
            for kc in range(KC):
                k.mm(pn[:, :n], ones[:], sq[:, kc, :n], start=(kc == 0), stop=(kc == KC - 1))
            k.ts("dve", rstd[:, :n], pn[:, :n], 1e-6, None, ALU.add)
            k.act(rstd[:, :n], rstd[:, :n], AF.Sqrt)
            k.recip(rstd[:, :n], rstd[:, :n])
            h = hT[b]
            k.tt("dve", h[:, :, :n], xb[b][:, :, :n], rstd[:, :n].unsqueeze(1).to_broadcast([128, KC, n]), ALU.mult)
            k.tt("pool", h[:, :, :n], h[:, :, :n], g1[:, :, s:s + 1].to_broadcast([128, KC, n]), ALU.mult)
            k.tt("dve", h[:, :, :n], h[:, :, :n], shf[:, :, s:s + 1].to_broadcast([128, KC, n]), ALU.add)
            for ct in range(NCT):
                wb = wbuf[wi % 3]; wi += 1
                k.dma(wb[:, 0:8, :], I["wA"][ct, :, 0:8, :], eng="sp", wk=0)
                k.dma(wb[:, 8:16, :], I["wA"][ct, :, 8:16, :], eng="act", wk=1)
                if ct == 9:
                    for sub in range(n // 128):
                        ps = pp[si % 3]; st = stg[si % 3]; si += 1
                        for kc in range(KC):
                            k.mm(ps[:, :128], h[:, kc, sub * 128:(sub + 1) * 128], wb[:, kc, :],
                                 start=(kc == 0), stop=(kc == KC - 1))
                        k.copy("dve", st[:, :128], ps[:, :128])
                        k.dma(Vi[t0 + sub * 128:t0 + (sub + 1) * 128, :], st[:, :128], eng="pool", wk=ti)
                    continue
                ps = pp[si % 3]; st = stg[si % 3]; si += 1
                for kc in range(KC):
                    k.mm(ps[:, :n], wb[:, kc, :], h[:, kc, :n], start=(kc == 0), stop=(kc == KC - 1))
                if ct >= NG0:
                    k.act(st[:, :n], ps[:, :n], AF.Sigmoid)
                    k.dma(O["gT"][(ct - NG0) * 128:(ct - NG0 + 1) * 128, t0:t0 + n], st[:, :n], eng="pool")
                elif ct == 10:
                    k.copy("dve", st[:, :n], ps[:, :n])
                    k.dma(O["ogT"][:, t0:t0 + n], st[0:64, :n], eng="pool")
                    k.dma(P[P_LG:P_LG + 64, t0:t0 + n], st[64:128, :n], eng="pool", wk=ti)
                else:
                    row = {0: P_A, 1: P_R, 2: P_V, 3: P_X, 4: P_X + 128, 5: P_X + 256, 6: P_X + 384, 7: P_Q, 8: P_F, 11: P_F2}[ct]
                    if ct % 2:
                        k.copy("dve", st[:, :n], ps[:, :n])
                    else:
                        k.copy("act", st[:, :n], ps[:, :n])
                    k.dma(P[row:row + 128, t0:t0 + n], st[:, :n], eng="pool", wk=ti)
    k.barrier()


A_IN = lambda Tx: {"xT": [128, KC, Tx], "wA": [NCT, 128, KC, 128], "vecD": [128, KC, 5],
                   "rwv": [64, 16], "rwx": [128, 4, 3], "rw1": [128, 4, 3, 64], "rw2": [64, 3, 64],
                   "lruv": [64, 16], "lruw": [64, 4, 64], "hgv": [128, 2], "cprime": [1, 64]}
A_OUT = lambda Tx: {"gT": [1024, Tx], "ogT": [64, Tx], "yA": [64, Tx], "yB": [64, Tx], "yC": [64, Tx],
                    "yD": [64, Tx]}


def build_A(layer, LCx=LC, LLx=LL, stages=("proj", "fnet", "lru", "hgrn", "rwkv"), debug=False):
    Tx = LCx + LLx
    nc = bass.Bass("TRN2", target_bir_lowering=False)
    k = KB(nc)
    outs = dict(A_OUT(Tx))
    if debug:
        outs["Pdbg"] = [P_ROWS, Tx]; outs["Vdbg"] = [Tx, 128]
    I, O = _mk(A_IN(Tx), outs, nc)
    if debug:
        P = O["Pdbg"]; Vi = O["Vdbg"]
    else:
        P = k.dram("Pscr", [P_ROWS, Tx]).ap(); Vi = k.dram("Viscr", [Tx, 128]).ap()
    if "proj" in stages:
        a_stage_proj(k, I, O, P, Vi, LCx, LLx)
    if "fnet" in stages:
        a_stage_fnet(k, I, O, P, LCx, LLx)
    if "lru" in stages:
        a_stage_lru(k, I, O, P, LCx, LLx)
    if "hgrn" in stages:
        a_stage_hgrn(k, I, O, P, Vi, LCx, LLx, layer)
    if "rwkv" in stages:
        a_stage_rwkv(k, I, O, P, LCx, LLx)
    k.emit(); k.close()
    return nc


def a_host_inputs(inp, l, j, xT_full, m):
    w_in = inp["w_in"][l]
    tiles = a_colspec(j)
    wA = np.empty((NCT, 128, KC, 128), np.float32)
    for ci, pieces in enumerate(tiles):
        cols = np.concatenate([np.arange(s, s + n) for s, n in pieces])
        wA[ci] = w_in[:, cols].reshape(KC, 128, 128).transpose(1, 0, 2)
    mm = m[l].reshape(2, 6, D)
    vecD = np.stack([fm(inp["norm_mix_g"][l]), fm(mm[0, 0]), fm(mm[0, 1]), fm(mm[1, 0]), fm(mm[1, 1])], axis=-1)
    hs = slice(64 * j, 64 * j + 64)
    rwv = np.zeros((64, 16), np.float32)
    mu = inp["rw_mu"][l]
    cols = [mu[0, hs], mu[1, hs], mu[2, hs], inp["rw_w0"][l, 0, hs], inp["rw_w0"][l, 1, hs],
            inp["rw_a0"][l, 0, hs], inp["rw_a0"][l, 1, hs], inp["rw_kk"][l, hs], inp["rw_ka"][l, hs],
            inp["rw_rk"][l, j], inp["rw_ln_g"][l, hs], inp["rw_ln_b"][l, hs]]
    for i, c in enumerate(cols):
        rwv[:, i] = c
    rwx = np.ascontiguousarray(mu[3:6].reshape(3, 4, 128).transpose(2, 1, 0))
    w1 = inp["rw_w1"][l]; a1 = inp["rw_a1"][l]; g1 = inp["rw_g1"][l]
    rw1 = np.stack([np.concatenate([w1[0], w1[1]], axis=1), np.concatenate([a1[0], a1[1]], axis=1), g1], axis=1)
    rw1 = np.ascontiguousarray(rw1.reshape(4, 128, 3, 64).transpose(1, 0, 2, 3))
    w2 = inp["rw_w2"][l]; a2 = inp["rw_a2"][l]; g2 = inp["rw_g2"][l]
    rw2 = np.stack([np.concatenate([w2[0][:, hs], w2[1][:, hs]], axis=0),
                    np.concatenate([a2[0][:, hs], a2[1][:, hs]], axis=0), g2[:, hs]], axis=1)
    lruv = np.zeros((64, 16), np.float32)
    cw = inp["lru_conv_w"][l]
    cols = [cw[0, hs], cw[1, hs], cw[2, hs], cw[3, hs], inp["lru_conv_b"][l, hs],
            inp["lru_ba"][l, 0, hs], inp["lru_ba"][l, 1, hs], inp["lru_bx"][l, 0, hs], inp["lru_bx"][l, 1, hs],
            inp["lru_lam"][l, 0, hs], inp["lru_lam"][l, 1, hs]]
    for i, c in enumerate(cols):
        lruv[:, i] = c
    lruw = np.stack([inp["lru_wa"][l, 0, j], inp["lru_wa"][l, 1, j], inp["lru_wx"][l, 0, j], inp["lru_wx"][l, 1, j]], axis=1)
    h = j // 2
    hgv = np.ascontiguousarray(inp["hg_lb_logits"][:, 128 * h:128 * h + 128].T)
    cprime = (np.arange(64) + 64 * (j % 2)).astype(np.float32)[None, :]
    return {"xT": xT_full, "wA": wA, "vecD": np.ascontiguousarray(vecD), "rwv": rwv, "rwx": rwx, "rw1": rw1,
            "rw2": np.ascontiguousarray(rw2), "lruv": lruv, "lruw": np.ascontiguousarray(lruw), "hgv": hgv,
            "cprime": cprime}


def dir_tiles(LCx, LLx, n, d):
    ts_ = seq_tiles(LCx, LLx, n)
    if d == 0:
        return ts_
    c = [t for t in ts_ if t[2] == 1][::-1]
    l_ = [t for t in ts_ if t[2] == 0][::-1]
    return c + l_


def a_stage_lru(k, I, O, P, LCx, LLx):
    Tx = LCx + LLx
    with k.scope():
        lv = k.sb([64, 16]); k.dma(lv[:], I["lruv"])
        lw = k.sb([64, 4, 64]); k.dma(lw[:], I["lruw"], eng="act")
        cd = k.sb([64, 2])
        k.act(cd[:], lv[:, 9:11], AF.Exp, scale=-1.0)
        k.act(cd[:], cd[:], AF.Ln, bias=1.0)
        k.ts("dve", cd[:], cd[:], -8.0, None, ALU.mult)
        u0 = k.sb([64, Tx]); xc = k.sb([64, Tx]); hs = k.sb([64, Tx])
        k.dma(u0[:], P[P_LX:P_LX + 64, :])
        for (s0, L) in ((0, LCx), (LCx, LLx)):
            k.ts("dve", xc[:, s0:s0 + L], u0[:, s0:s0 + L], lv[:, 2:3], lv[:, 4:5], ALU.mult, ALU.add)
            k.stt(xc[:, s0 + 2:s0 + L], u0[:, s0:s0 + L - 2], lv[:, 0:1], xc[:, s0 + 2:s0 + L], ALU.mult, ALU.add)
            k.stt(xc[:, s0 + 1:s0 + L], u0[:, s0:s0 + L - 1], lv[:, 1:2], xc[:, s0 + 1:s0 + L], ALU.mult, ALU.add)
            k.stt(xc[:, s0:s0 + L - 1], u0[:, s0 + 1:s0 + L], lv[:, 3:4], xc[:, s0:s0 + L - 1], ALU.mult, ALU.add)
        pr = [k.ps([64, 512]) for _ in range(2)]; pi = [k.ps([64, 512]) for _ in range(2)]
        rr = [k.sb([64, 512]) for _ in range(2)]; ii = [k.sb([64, 512]) for _ in range(2)]
        aa = [k.sb([64, 512]) for _ in range(2)]; mmv = [k.sb([64, 512]) for _ in range(2)]
        hh = [k.sb([64, 512]) for _ in range(2)]
        carry = k.sb([64, 2])
        for d in range(2):
            first = True
            for ti, (t0, n, c) in enumerate(dir_tiles(LCx, LLx, 512, d)):
                b = ti % 2
                xs = xc[:, t0:t0 + n]
                k.mm(pr[b][:, :n], lw[:, d, :], xs)
                k.mm(pi[b][:, :n], lw[:, 2 + d, :], xs)
                k.act(rr[b][:, :n], pr[b][:, :n], AF.Sigmoid, bias=lv[:, 5 + d:6 + d])
                k.act(ii[b][:, :n], pi[b][:, :n], AF.Sigmoid, bias=lv[:, 7 + d:8 + d])
                k.act(aa[b][:, :n], rr[b][:, :n], AF.Exp, scale=cd[:, d:d + 1])
                k.tt("pool", mmv[b][:, :n], aa[b][:, :n], aa[b][:, :n], ALU.mult)
                k.ts("pool", mmv[b][:, :n], mmv[b][:, :n], -1.0, 1.0, ALU.mult, ALU.add)
                k.act(mmv[b][:, :n], mmv[b][:, :n], AF.Sqrt)
                k.tt("pool", ii[b][:, :n], ii[b][:, :n], mmv[b][:, :n], ALU.mult)
                k.tt("dve", ii[b][:, :n], ii[b][:, :n], xs, ALU.mult)
                init = 0.0 if first else carry[:, d:d + 1]
                if d == 0:
                    k.scan(hh[b][:, :n], aa[b][:, :n], ii[b][:, :n], init, ALU.mult, ALU.add)
                    k.copy("dve", carry[:, 0:1], hh[b][:, n - 1:n], wk=0, rk=None)
                    k.copy("pool", hs[:, t0:t0 + n], hh[b][:, :n], wk=(t0))
                else:
                    k.scan(hh[b][:, n - 1::-1] if n < 512 else hh[b][:, ::-1],
                           aa[b][:, n - 1::-1] if n < 512 else aa[b][:, ::-1],
                           ii[b][:, n - 1::-1] if n < 512 else ii[b][:, ::-1], init, ALU.mult, ALU.add)
                    k.copy("dve", carry[:, 1:2], hh[b][:, 0:1], wk=1, rk=None)
                    k.tt("pool", hs[:, t0:t0 + n], hs[:, t0:t0 + n], hh[b][:, :n], ALU.add, wk=(t0), rk={hs.name: t0})
                first = False
        k.dma(u0[:], P[P_LG:P_LG + 64, :])
        x2 = [k.sb([64, 2048]) for _ in range(2)]
        for ti, t0 in enumerate(range(0, Tx, 2048)):
            n = min(2048, Tx - t0); b = ti % 2
            xs = u0[:, t0:t0 + n]; w = x2[b][:, :n]
            k.act(w, xs, AF.Square)
            k.ts("dve", w, w, 0.044715, 1.0, ALU.mult, ALU.add)
            k.tt("dve", w, w, xs, ALU.mult)
            k.act(w, w, AF.Tanh, scale=0.7978845608028654)
            k.ts("dve", w, w, 0.5, 0.5, ALU.mult, ALU.add)
            k.tt("pool", w, w, xs, ALU.mult)
            k.tt("dve", w, w, hs[:, t0:t0 + n], ALU.mult)
            k.dma(O["yD"][:, t0:t0 + n], w, eng="act")


def a_stage_hgrn(k, I, O, P, Vi, LCx, LLx, layer):
    Tx = LCx + LLx
    with k.scope():
        hv = k.sb([128, 2]); k.dma(hv[:], I["hgv"])
        lb = k.sb([128, 1]); oml = k.sb([128, 1])
        if layer == 0:
            k.memset("dve", lb[:], 0.0); k.memset("dve", oml[:], 1.0)
        else:
            k.tt("dve", lb[:], hv[:, 1:2], hv[:, 0:1], ALU.subtract)
            k.act(lb[:], lb[:], AF.Sigmoid)
            k.ts("dve", oml[:], lb[:], -1.0, 1.0, ALU.mult, ALU.add)
        ident = k.sb([128, 128]); k.memset("dve", ident[:], 1.0)
        k.aselect(ident[:], [[-1, 128]], ALU.is_equal, 0, 1)
        M8 = k.sb([128, 8]); k.memset("dve", M8[:], 1.0)
        k.aselect(M8[:], [[-16, 8]], ALU.is_ge, 0, 1)
        k.aselect(M8[:], [[16, 8]], ALU.is_ge, 15, -1)
        MASK = [k.sb([128, 128]) for _ in range(2)]
        for d in range(2):
            mk_ = MASK[d]
            k.memset("dve", mk_[:], 1.0)
            k.aselect(mk_[:], [[-16, 8], [0, 16]], ALU.is_ge, 0, 1)
            k.aselect(mk_[:], [[16, 8], [0, 16]], ALU.is_ge, 15, -1)
            if d == 0:
                k.aselect(mk_[:], [[16, 8], [1, 16]], ALU.is_ge, 0, -1)
            else:
                k.aselect(mk_[:], [[-16, 8], [-1, 16]], ALU.is_ge, 0, 1)
        cm = [k.sb([128, 512]) for _ in range(2)]
        k.memset("dve", cm[0][:], 1.0); k.memset("dve", cm[0][:, 0::16], 0.0)
        k.memset("dve", cm[1][:], 1.0); k.memset("dve", cm[1][:, 15::16], 0.0)
        ysum = k.sb([64, Tx]); k.memset("pool", ysum[:], 0.0)
        S = [k.sb([128, 128]) for _ in range(2)]
        for d in range(2):
            k.memset("dve", S[d][:], 0.0)
        nb = 2
        zt = [[k.sb([128, 512]) for _ in range(nb)] for d in range(2)]
        qt = [[k.sb([128, 512]) for _ in range(nb)] for d in range(2)]
        kh = [[k.sb([128, 512]) for _ in range(nb)] for d in range(2)]
        Gt = [[k.sb([128, 512]) for _ in range(nb)] for d in range(2)]
        kb_ = [[k.sb([128, 512]) for _ in range(nb)] for d in range(2)]
        eGl = [[k.sb([128, 32]) for _ in range(nb)] for d in range(2)]
        Vt = [[k.sb([128, 4, 128]) for _ in range(nb)] for d in range(2)]
        Ktm = [k.sb([128, 128]) for d in range(2)]
        Km = [k.sb([128, 8, 128]) for d in range(2)]
        Am = [k.sb([128, 128]) for d in range(2)]
        Xs = [k.sb([128, 128]) for d in range(2)]
        pb1 = [k.ps([128, 512]) for d in range(2)]
        pb2 = [k.ps([128, 512]) for d in range(2)]
        pb3 = [k.ps([128, 512]) for d in range(2)]
        blocks = [dir_tiles(LCx, LLx, 512, d) for d in range(2)]
        nblk = len(blocks[0])

        def pre(d, bi):
            t0, n, c = blocks[d][bi]
            b = bi % nb
            z = zt[d][b][:, :n]; q = qt[d][b][:, :n]; kk_ = kh[d][b][:, :n]; G = Gt[d][b][:, :n]; KB_ = kb_[d][b][:, :n]
            nch = n // 16
            k.dma(z, P[(P_F if d == 0 else P_F2):(P_F if d == 0 else P_F2) + 128, t0:t0 + n], eng="sp")
            k.dma(q, P[P_Q:P_Q + 128, t0:t0 + n], eng="act")
            k.dma(Vt[d][b][:, :n // 128, :], Vi[t0:t0 + n, :].rearrange("(a s) c -> s a c", s=128), eng="pool")
            k.act(z, z, AF.Sigmoid)
            k.ts("dve", z, z, oml[:, 0:1], lb[:, 0:1], ALU.mult, ALU.add)
            k.act(G, z, AF.Ln)
            k.ts("pool", z, z, -1.0, 1.0, ALU.mult, ALU.add)
            if d == 0:
                k.scan(G, cm[0][:, :n], G, 0.0, ALU.mult, ALU.add)
            else:
                k.scan(Gt[d][b][:, n - 1::-1] if n < 512 else Gt[d][b][:, ::-1],
                       cm[1][:, n - 1::-1] if n < 512 else cm[1][:, ::-1],
                       Gt[d][b][:, n - 1::-1] if n < 512 else Gt[d][b][:, ::-1], 0.0, ALU.mult, ALU.add)
            k.act(q, q, AF.Silu)
            k.act(kk_, G, AF.Exp)
            k.tt("dve", q, q, kk_, ALU.mult)
            k.act(kk_, G, AF.Exp, scale=-1.0)
            k.tt("dve", kk_, kk_, z, ALU.mult)
            gl = Gt[d][b][:, 15:n:16] if d == 0 else Gt[d][b][:, 0:n:16]
            k.act(eGl[d][b][:, :nch], gl, AF.Exp)
            k.tt("pool", kb_[d][b][:, :n].rearrange("p (c i) -> p c i", i=16),
                 kh[d][b][:, :n].rearrange("p (c i) -> p c i", i=16),
                 eGl[d][b][:, :nch].unsqueeze(2).to_broadcast([128, nch, 16]), ALU.mult)

        def chain(d, bi):
            t0, n, c = blocks[d][bi]
            b = bi % nb
            nt = n // 128
            subs = range(nt) if d == 0 else range(nt - 1, -1, -1)
            for sub in subs:
                cs = slice(sub * 128, (sub + 1) * 128)
                pT = pb1[d][:, 0:128]; pA = pb1[d][:, 128:256]; pU = pb3[d][:, 0:128]
                pX = pb2[d][:, 0:128]; pY = pb2[d][:, 128:256]
                k.tr(pT, kb_[d][b][:, cs], ident[:])
                k.copy("act", Ktm[d][:], pT)
                k.tt("pool", Km[d][:], Ktm[d][:].unsqueeze(1).to_broadcast([128, 8, 128]),
                     M8[:].unsqueeze(2).to_broadcast([128, 8, 128]), ALU.mult)
                k.mm(pA, kh[d][b][:, cs], qt[d][b][:, cs])
                k.tt("dve", Am[d][:], pA, MASK[d][:], ALU.mult)
                k.mm(pX, Vt[d][b][:, sub, :], Am[d][:])
                k.copy("act", Xs[d][:], pX)
                chs = range(8) if d == 0 else range(7, -1, -1)
                for ch in chs:
                    c0 = sub * 128 + ch * 16
                    k.mm(pb2[d][:, 128 + ch * 16:128 + (ch + 1) * 16], S[d][:], qt[d][b][:, c0:c0 + 16])
                    k.mm(pU, Km[d][:, ch, :], Vt[d][b][:, sub, :])
                    gi = sub * 8 + ch
                    k.stt(S[d][:], S[d][:], eGl[d][b][:, gi:gi + 1], pU, ALU.mult, ALU.add)
                tt0 = t0 + sub * 128
                k.tt("dve", Xs[d][:], Xs[d][:], pY, ALU.add)
                k.tt("pool", ysum[:, tt0:tt0 + 128], ysum[:, tt0:tt0 + 128], Xs[d][0:64, :], ALU.add, wk=tt0, rk={ysum.name: tt0})

        for d in range(2):
            pre(d, 0)
        for bi in range(nblk):
            for d in range(2):
                if bi + 1 < nblk:
                    pre(d, bi + 1)
            for d in range(2):
                if HG_DEBUG != "pre":
                    chain(d, bi)
        k.dma(O["yC"], ysum[:])


def k_frac(k, out, y, ti, tf, e1="pool", e2="dve"):
    k.copy(e1, ti, y)
    k.copy(e1, tf, ti)
    k.tt(e2, out, y, tf, ALU.subtract)


HALF_PI_SAFE = 1.5707962


def a_stage_fnet(k, I, O, P, LCx, LLx):
    with k.scope():
        cp = k.sb([128, 64]); k.dma(cp[:], I["cprime"].broadcast_to([128, 64]))
        pi_i = k.sb([128, 1], I32); pidx = k.sb([128, 1])
        k.iota(pi_i[:], [[0, 1]], 0, 1)
        k.copy("dve", pidx[:], pi_i[:])
        yy = k.sb([128, 64]); yi = k.sb([128, 64], I32); yf = k.sb([128, 64]); CS = k.sb([128, 128])
        k.ts("dve", yy[:], cp[:], pidx[:, 0:1], 1.0 / 128.0, ALU.mult, ALU.mult)
        k_frac(k, yy[:], yy[:], yi[:], yf[:])
        k.act(CS[:, 64:128], yy[:], AF.Sin, scale=-TWO_PI_SAFE)
        k.act(yf[:], yy[:], AF.Abs)
        k.act(CS[:, 0:64], yf[:], AF.Sin, scale=-TWO_PI_SAFE, bias=HALF_PI_SAFE)
        pq_ps = k.ps([128, 128])
        for (s0, L) in ((0, LCx), (LCx, LLx)):
            with k.scope():
                nch = L // 128
                Lh = min(L, 2048)
                TB = min(Lh, 512)
                ntb = Lh // TB
                U = k.sb([128, L]); k.dma(U[:], P[P_A:P_A + 128, s0:s0 + L])
                PQ = k.sb([128, nch, 128])
                for a in range(nch):
                    k.mm(pq_ps[:], U[:, a * 128:(a + 1) * 128], CS[:])
                    k.copy("dve" if a % 2 else "act", PQ[:, a, :], pq_ps[:], wk=a)
                tp_i = k.sb([128, Lh], I32); tp = k.sb([128, Lh])
                f1 = k.sb([128, Lh]); stp = k.sb([128, Lh]); g = k.sb([128, Lh]); tmp = k.sb([128, Lh])
                ti = k.sb([128, Lh], I32); tf = k.sb([128, Lh]); ab = k.sb([128, Lh])
                Ss = [k.sb([128, Lh]) for _ in range(2)]; Cc = [k.sb([128, Lh]) for _ in range(2)]
                Yp = [k.ps([64, 512]) for _ in range(ntb)]
                yo = [k.sb([64, 512]) for _ in range(2)]
                scale = float(1.0 / np.sqrt(L * 128.0))
                for h0 in range(0, L, Lh):
                    k.iota(tp_i[:], [[1, Lh]], h0, 0)
                    k.copy("dve", tp[:], tp_i[:])
                    k.ts("dve", f1[:], tp[:], pidx[:, 0:1], 1.0 / L, ALU.mult, ALU.mult)
                    k_frac(k, g[:], f1[:], ti[:], tf[:])
                    k.ts("dve", stp[:], tp[:], 128.0 / L, None, ALU.mult)
                    k_frac(k, stp[:], stp[:], ti[:], tf[:])
                    for a in range(nch):
                        b = a % 2
                        if a > 0:
                            k.tt("dve", tmp[:], g[:], stp[:], ALU.add)
                            k_frac(k, g[:], tmp[:], ti[:], tf[:])
                        k.act(Ss[b][:], g[:], AF.Sin, scale=TWO_PI_SAFE)
                        k.act(ab[:], g[:], AF.Abs)
                        k.act(Cc[b][:], ab[:], AF.Sin, scale=-TWO_PI_SAFE, bias=HALF_PI_SAFE)
                        for tb in range(ntb):
                            k.mm(Yp[tb][:, :TB], PQ[:, a, 0:64], Cc[b][:, tb * TB:(tb + 1) * TB], start=(a == 0), stop=False,
                                 rk={PQ.name: a})
                            k.mm(Yp[tb][:, :TB], PQ[:, a, 64:128], Ss[b][:, tb * TB:(tb + 1) * TB], start=False,
                                 stop=(a == nch - 1), rk={PQ.name: a})
                    for tb in range(ntb):
                        k.ts("dve", yo[tb % 2][:, :TB], Yp[tb][:, :TB], scale, None, ALU.mult)
                        k.dma(O["yA"][:, s0 + h0 + tb * TB:s0 + h0 + (tb + 1) * TB], yo[tb % 2][:, :TB], eng="sp")


RW_R, RW_KK, RW_W0, RW_W1, RW_NB0, RW_NB1, RW_KT0, RW_KT1, RW_G, RW_BV = range(10)


def a_stage_rwkv(k, I, O, P, LCx, LLx):
    Tx = LCx + LLx
    RW = k.dram("RWscr", [10, 64, Tx]).ap()
    Vtm = k.dram("Vtmscr", [Tx, 64]).ap()
    seq_end = {0: LCx, LCx: Tx}
    with k.scope():
        rv = k.sb([64, 16]); k.dma(rv[:], I["rwv"])
        rx = k.sb([128, 4, 3]); k.dma(rx[:], I["rwx"], eng="act")
        r1 = k.sb([128, 4, 3, 64]); k.dma(r1[:], I["rw1"], eng="act")
        r2 = k.sb([64, 3, 64]); k.dma(r2[:], I["rw2"])
        omka = k.sb([64, 1]); k.ts("dve", omka[:], rv[:, 8:9], -1.0, 1.0, ALU.mult, ALU.add)
        ones64 = k.sb([64, 64]); k.memset("dve", ones64[:], 1.0)
        ident = k.sb([64, 64]); k.memset("dve", ident[:], 1.0)
        k.aselect(ident[:], [[-1, 64]], ALU.is_equal, 0, 1)
        N = 512
        u3 = k.sb([64, 3, N + 2])
        ux = k.sb([128, 4, N + 2])
        t3 = k.sb([64, 3, N]); rkv = k.sb([64, 3, N])
        tx = k.sb([128, 4, N]); xxx = k.sb([128, 4, N]); xm = [k.sb([128, 4, N]) for _ in range(3)]
        lw_ = k.sb([64, N]); la_ = k.sb([64, N]); lg_ = k.sb([64, N])
        wd = [k.sb([64, N]) for _ in range(2)]; ad = [k.sb([64, N]) for _ in range(2)]
        ktd = [k.sb([64, N]) for _ in range(2)]; nbd = [k.sb([64, N]) for _ in range(2)]
        gg = k.sb([64, N]); kkk = k.sb([64, N]); sq = k.sb([64, N]); kkn = k.sb([64, N]); bv = k.sb([64, N])
        vt = k.sb([128, 4, 64])
        pl = [k.ps([64, 512]) for _ in range(3)]; pu = [k.ps([64, 512]) for _ in range(2)]
        pv = k.ps([128, 512])
        for ti_, (t0, n, c) in enumerate(seq_tiles(LCx, LLx, N)):
            s0 = 0 if c == 1 else LCx
            e0 = seq_end[s0]
            lo = t0 - 1 if t0 > s0 else t0
            hi = t0 + n + 1 if t0 + n < e0 else t0 + n
            o = 1 - (t0 - lo)
            if t0 == s0:
                k.memset("dve", u3[:, :, 0:1], 0.0); k.memset("pool", ux[:, :, 0:1], 0.0)
            if t0 + n == e0:
                k.memset("dve", u3[:, :, n + 1:n + 2], 0.0); k.memset("pool", ux[:, :, n + 1:n + 2], 0.0)
            w_ = hi - lo
            k.dma(u3[:, 0:2, o:o + w_], P[P_R:P_R + 128, lo:hi].rearrange("(c p) t -> p c t", p=64), eng="sp")
            k.dma(u3[:, 2, o:o + w_], P[P_V:P_V + 64, lo:hi], eng="act")
            k.dma(ux[:, :, o:o + w_], P[P_X:P_X + 512, lo:hi].rearrange("(c p) t -> p c t", p=128), eng="pool")
            k.tt("dve", t3[:, :, :n], u3[:, :, 0:n], u3[:, :, 2:n + 2], ALU.add)
            k.stt(t3[:, :, :n], t3[:, :, :n], 0.5, u3[:, :, 1:n + 1], ALU.mult, ALU.subtract)
            for i in range(3):
                k.stt(rkv[:, i, :n], t3[:, i, :n], rv[:, i:i + 1], u3[:, i, 1:n + 1], ALU.mult, ALU.add)
            k.tt("pool", tx[:, :, :n], ux[:, :, 0:n], ux[:, :, 2:n + 2], ALU.add)
            k.stt(xxx[:, :, :n], tx[:, :, :n], 0.5, ux[:, :, 1:n + 1], ALU.mult, ALU.subtract)
            for i in range(3):
                k.tt("pool", xm[i][:, :, :n], xxx[:, :, :n], rx[:, :, i:i + 1].to_broadcast([128, 4, n]), ALU.mult)
                k.tt("dve" if i == 1 else "pool", xm[i][:, :, :n], xm[i][:, :, :n], ux[:, :, 1:n + 1], ALU.add)
            for i in range(3):
                for cc_ in range(4):
                    k.mm(pl[i][:, :n], r1[:, cc_, i, :], xm[i][:, cc_, :n], start=(cc_ == 0), stop=(cc_ == 3))
            k.act(lw_[:, :n], pl[0][:, :n], AF.Tanh)
            k.copy("dve", la_[:, :n], pl[1][:, :n])
            k.act(lg_[:, :n], pl[2][:, :n], AF.Sigmoid)
            R_, K_, V_ = rkv[:, 0, :n], rkv[:, 1, :n], rkv[:, 2, :n]
            for d in range(2):
                k.mm(pu[0][:, :n], r2[32 * d:32 * d + 32, 0, :], lw_[32 * d:32 * d + 32, :n])
                k.act(wd[d][:, :n], pu[0][:, :n], AF.Sigmoid, bias=rv[:, 3 + d:4 + d])
                k.act(wd[d][:, :n], wd[d][:, :n], AF.Exp, scale=-float(np.exp(-0.5)))
                k.mm(pu[1][:, :n], r2[32 * d:32 * d + 32, 1, :], la_[32 * d:32 * d + 32, :n])
                k.act(ad[d][:, :n], pu[1][:, :n], AF.Sigmoid, bias=rv[:, 5 + d:6 + d])
            k.mm(pu[0][:, :n], r2[:, 2, :], lg_[:, :n])
            k.copy("act", gg[:, :n], pu[0][:, :n])
            k.ts("dve", kkk[:, :n], K_, rv[:, 7:8], None, ALU.mult)
            k.act(sq[:, :n], kkk[:, :n], AF.Square)
            k.mm(pu[1][:, :n], ones64[:], sq[:, :n])
            k.act(sq[:, :n], pu[1][:, :n], AF.Sqrt)
            k.ts("dve", sq[:, :n], sq[:, :n], 1e-12, None, ALU.max)
            k.recip(sq[:, :n], sq[:, :n])
            k.tt("dve", kkn[:, :n], kkk[:, :n], sq[:, :n], ALU.mult)
            for d in range(2):
                k.ts("dve", ktd[d][:, :n], ad[d][:, :n], rv[:, 8:9], omka[:, 0:1], ALU.mult, ALU.add)
                k.tt("dve", ktd[d][:, :n], ktd[d][:, :n], K_, ALU.mult)
                k.stt(nbd[d][:, :n], ad[d][:, :n], -1.0, kkn[:, :n], ALU.mult, ALU.mult)
            k.tt("pool", bv[:, :n], ktd[0][:, :n], ktd[1][:, :n], ALU.add)
            k.stt(bv[:, :n], bv[:, :n], rv[:, 9:10], R_, ALU.mult, ALU.mult)
            k.mm(pu[0][:, :n], ones64[:], bv[:, :n])
            k.tt("dve", bv[:, :n], pu[0][:, :n], V_, ALU.mult)
            for sub in range(n // 128):
                k.tr(pv[:, sub * 64:(sub + 1) * 64], rkv[:, 2, sub * 128:(sub + 1) * 128], ident[:])
            k.copy("act", vt[:, :n // 128, :], pv[:, :(n // 128) * 64].rearrange("p (a c) -> p a c", c=64))
            k.dma(Vtm[t0:t0 + n, :].rearrange("(a s) c -> s a c", s=128), vt[:, :n // 128, :], eng="sp", wk=ti_)
            for idx, src in ((RW_R, R_), (RW_KK, kkn[:, :n]), (RW_W0, wd[0][:, :n]), (RW_W1, wd[1][:, :n]),
                             (RW_NB0, nbd[0][:, :n]), (RW_NB1, nbd[1][:, :n]), (RW_KT0, ktd[0][:, :n]),
                             (RW_KT1, ktd[1][:, :n]), (RW_G, gg[:, :n]), (RW_BV, bv[:, :n])):
                k.dma(RW[idx, :, t0:t0 + n], src, eng=("act" if idx % 2 else "pool"), wk=(idx, ti_))
    outer = k.scope(); outer.__enter__()
    ysum = k.sb([64, Tx]); k.memset("pool", ysum[:], 0.0)
    with k.scope():
        SEG = 256
        ident = k.sb([128, 128]); k.memset("dve", ident[:], 1.0)
        k.aselect(ident[:], [[-1, 128]], ALU.is_equal, 0, 1)
        ST = [k.sb([64, 64]) for _ in range(2)]; T1 = [k.sb([64, 64]) for _ in range(2)]
        for d in range(2):
            k.memset("dve", ST[d][:], 0.0)
        segs = [dir_tiles(LCx, LLx, SEG, d) for d in range(2)]
        nseg = len(segs[0])
        sv = [[k.sb([64, 5, SEG]) for _ in range(2)] for d in range(2)]
        vtm = [[k.sb([128, 2, 64]) for _ in range(2)] for d in range(2)]
        Z = [[k.sb([64, 8, 64]) for _ in range(2)] for d in range(2)]
        p_sa = [k.ps([64, 64]) for d in range(2)]
        p_y = [k.ps([64, SEG]) for d in range(2)]
        p_vb = [[k.ps([64, 8, 64]) for _ in range(2)] for d in range(2)]
        ye = [k.sb([64, SEG]) for d in range(2)]

        def load(d, si):
            t0, n, c = segs[d][si]
            b = si % 2
            for q, idx in enumerate((RW_R, RW_KK, RW_W0 + d, RW_NB0 + d, RW_KT0 + d)):
                k.dma(sv[d][b][:, q, :], RW[idx, :, t0:t0 + n], eng=("sp", "act", "pool")[q % 3], wk=q)
            k.dma(vtm[d][b][:], Vtm[t0:t0 + n, :].rearrange("(a s) c -> s a c", s=128), eng="sp")

        for d in range(2):
            load(d, 0)
        for si in range(nseg):
            for d in range(2):
                if si + 1 < nseg:
                    load(d, si + 1)
            b = si % 2
            for g8 in range(SEG // 8 + 1):
                for d in range(2):
                    if g8 < SEG // 8:
                        vb = p_vb[d][g8 % 2]; zz = Z[d][g8 % 2]
                        cols = []
                        for jj in range(8):
                            i = g8 * 8 + jj
                            c = i if d == 0 else SEG - 1 - i
                            cols.append(c)
                            k.mm(vb[:, jj, :], ident[:, c % 128:c % 128 + 1].to_broadcast([128, 64]), vtm[d][b][:, c // 128, :])
                        for jj in range(8):
                            c = cols[jj]
                            k.act(zz[:, jj, :], vb[:, jj, :], AF.Copy, scale=sv[d][b][:, 4, c:c + 1], wk=jj,
                                  rk={sv[d][b].name: 4})
                for jj in range(8):
                    for d in range(2):
                        gp = g8 - 1
                        if gp < 0:
                            continue
                        i = gp * 8 + jj
                        c = i if d == 0 else SEG - 1 - i
                        s_ = sv[d][b]; zz = Z[d][gp % 2]
                        k.mm(p_sa[d][:], s_[:, 1, c:c + 1].to_broadcast([64, 64]), ST[d][:], rk={s_.name: 1})
                        k.stt(T1[d][:], p_sa[d][:], s_[:, 3, c:c + 1], zz[:, jj, :], ALU.mult, ALU.add,
                              rk={s_.name: 3, zz.name: jj})
                        k.stt(ST[d][:], ST[d][:], s_[:, 2, c:c + 1], T1[d][:], ALU.mult, ALU.add, rk={s_.name: 2})
                        k.mm(p_y[d][:, c:c + 1], ST[d][:], s_[:, 0, c:c + 1], rk={s_.name: 0})
            for d in range(2):
                t0, n, c = segs[d][si]
                k.copy("act", ye[d][:], p_y[d][:])
                k.tt("pool", ysum[:, t0:t0 + n], ysum[:, t0:t0 + n], ye[d][:], ALU.add, wk=t0, rk={ysum.name: t0})
    with k.scope():
        rv = k.sb([64, 16]); k.dma(rv[:], I["rwv"])
        o64 = k.sb([64, 64]); k.memset("dve", o64[:], 1.0 / 64.0)
        N = 512
        yc = k.sb([64, N]); sq = k.sb([64, N]); gt = [k.sb([64, N]) for _ in range(2)]; bt = [k.sb([64, N]) for _ in range(2)]
        pm = k.ps([64, N]); pvv = k.ps([64, N])
        for ti_, (t0, n, c) in enumerate(seq_tiles(LCx, LLx, N)):
            b = ti_ % 2
            k.dma(gt[b][:, :n], RW[RW_G, :, t0:t0 + n], eng="sp")
            k.dma(bt[b][:, :n], RW[RW_BV, :, t0:t0 + n], eng="act")
            k.mm(pm[:, :n], o64[:], ysum[:, t0:t0 + n], rk={ysum.name: None})
            k.tt("dve", yc[:, :n], ysum[:, t0:t0 + n], pm[:, :n], ALU.subtract, rk={ysum.name: None})
            k.act(sq[:, :n], yc[:, :n], AF.Square)
            k.mm(pvv[:, :n], o64[:], sq[:, :n])
            k.ts("dve", sq[:, :n], pvv[:, :n], 64e-5, None, ALU.add)
            k.act(sq[:, :n], sq[:, :n], AF.Sqrt)
            k.recip(sq[:, :n], sq[:, :n])
            k.tt("dve", yc[:, :n], yc[:, :n], sq[:, :n], ALU.mult)
            k.ts("dve", yc[:, :n], yc[:, :n], rv[:, 10:11], rv[:, 11:12], ALU.mult, ALU.add)
            k.tt("pool", yc[:, :n], yc[:, :n], bt[b][:, :n], ALU.add)
            k.tt("dve", yc[:, :n], yc[:, :n], gt[b][:, :n], ALU.mult)
            k.dma(O["yB"][:, t0:t0 + n], yc[:, :n], eng="pool")
    outer.__exit__(None, None, None)


def build_B(tiles):
    NL = sum(t[1] for t in tiles)
    nc = bass.Bass("TRN2", target_bir_lowering=False)
    k = KB(nc)
    I, O = _mk({"xT": [128, KC, NL], "brT": [128, KC, NL], "ogT": [128, 4, NL], "gT": [128, 64, NL],
                "wbr": [16, 128, KC, 128], "wout": [16, 128, KC, 128], "vecB": [128, KC, 8], "hgn": [128, 1],
                "rw": [128, KC, 64], "rb": [128, 64]},
               {"x1T": [128, KC, NL], "h2T": [128, KC, NL], "cw": [NL, 64]}, nc)
    vec = k.sb([128, KC, 8]); k.dma(vec[:], I["vecB"])
    hgn = k.sb([128, 1]); k.dma(hgn[:], I["hgn"], eng="act")
    rw = k.sb([128, KC, 64]); k.dma(rw[:], I["rw"], eng="act")
    rb = k.sb([128, 64]); k.dma(rb[:], I["rb"], eng="pool")
    g2 = k.sb([128, KC, 2])
    for s in range(2):
        k.ts("dve", g2[:, :, s], vec[:, :, 4 + 2 * s], 1.0, None, ALU.add, wk=s)
        k.tt("dve", g2[:, :, s], g2[:, :, s], vec[:, :, 2], ALU.mult, wk=s, rk={g2.name: s})
    onesD = k.sb([128, 128]); k.memset("dve", onesD[:], 1.0 / D)
    ones128 = k.sb([128, 128]); k.memset("dve", ones128[:], 1.0 / 128.0)
    N = 256
    xt = k.sb([128, KC, N]); br = k.sb([128, KC, N]); og = k.sb([128, 4, N]); mg = k.sb([128, KC, N])
    x1 = k.sb([128, KC, N]); sq = k.sb([128, KC, N])
    gt = [k.sb([128, 4, N]) for _ in range(2)]
    wb = [k.sb([128, KC, 128]) for _ in range(3)]
    tmp = [k.sb([128, N]) for _ in range(4)]
    rstd = k.sb([128, N])
    pk = [k.ps([128, N]) for _ in range(4)]
    pn = k.ps([128, N]); po = [k.ps([128, N]) for _ in range(2)]; pr = k.ps([128, 64])
    wi = 0
    for (c0, n, s) in tiles:
        k.dma(xt[:, :, :n], I["xT"][:, :, c0:c0 + n], eng="sp")
        k.dma(br[:, :, :n], I["brT"][:, :, c0:c0 + n], eng="act")
        k.dma(og[:, :, :n], I["ogT"][:, :, c0:c0 + n], eng="pool")
        k.act(sq[:, 0:4, :n], br[:, 8:12, :n], AF.Square)
        k.act(og[:, :, :n], og[:, :, :n], AF.Silu)
        for h in range(4):
            k.mm(pn[:, :n], ones128[:], sq[:, h, :n])
            k.ts("dve", rstd[:, :n], pn[:, :n], 1e-6, None, ALU.add)
            k.act(rstd[:, :n], rstd[:, :n], AF.Sqrt)
            k.recip(rstd[:, :n], rstd[:, :n])
            k.stt(br[:, 8 + h, :n], br[:, 8 + h, :n], hgn[:, 0:1], rstd[:, :n], ALU.mult, ALU.mult)
            k.tt("pool", br[:, 8 + h, :n], br[:, 8 + h, :n], og[:, h, :n], ALU.mult)
        gv = I["gT"].rearrange("p (k d) t -> p k d t", k=4)
        for dt in range(16):
            w = wb[wi % 3]; wi += 1
            k.dma(w[:], I["wbr"][dt], eng="sp")
            g = gt[dt % 2]
            k.dma(g[:, :, :n], gv[:, :, dt, c0:c0 + n], eng="act")
            for kb_i in range(4):
                for c in range(4):
                    k.mm(pk[kb_i][:, :n], w[:, kb_i * 4 + c, :], br[:, kb_i * 4 + c, :n], start=(c == 0), stop=(c == 3))
            k.tt("dve", mg[:, dt, :n], pk[0][:, :n], g[:, 0, :n], ALU.mult, wk=dt)
            for kb_i in range(1, 4):
                k.tt("dve", tmp[kb_i][:, :n], pk[kb_i][:, :n], g[:, kb_i, :n], ALU.mult)
                k.tt("pool", mg[:, dt, :n], mg[:, dt, :n], tmp[kb_i][:, :n], ALU.add, wk=dt, rk={mg.name: dt})
        for dt in range(16):
            w = wb[wi % 3]; wi += 1
            k.dma(w[:], I["wout"][dt], eng="sp")
            p = po[dt % 2]
            for kc in range(KC):
                k.mm(p[:, :n], w[:, kc, :], mg[:, kc, :n], start=(kc == 0), stop=(kc == KC - 1))
            k.stt(x1[:, dt, :n], p[:, :n], vec[:, dt, s:s + 1], xt[:, dt, :n], ALU.mult, ALU.add, wk=dt)
        k.dma(O["x1T"][:, :, c0:c0 + n], x1[:, :, :n], eng="pool")
        k.act(sq[:, :, :n], x1[:, :, :n], AF.Square)
        for kc in range(KC):
            k.mm(pn[:, :n], onesD[:], sq[:, kc, :n], start=(kc == 0), stop=(kc == KC - 1))
        k.ts("dve", rstd[:, :n], pn[:, :n], 1e-6, None, ALU.add)
        k.act(rstd[:, :n], rstd[:, :n], AF.Sqrt)
        k.recip(rstd[:, :n], rstd[:, :n])
        h2 = sq
        k.tt("dve", h2[:, :, :n], x1[:, :, :n], rstd[:, :n].unsqueeze(1).to_broadcast([128, KC, n]), ALU.mult)
        k.tt("pool", h2[:, :, :n], h2[:, :, :n], g2[:, :, s:s + 1].to_broadcast([128, KC, n]), ALU.mult)
        k.tt("dve", h2[:, :, :n], h2[:, :, :n], vec[:, :, 3 + 2 * s:4 + 2 * s].to_broadcast([128, KC, n]), ALU.add)
        k.dma(O["h2T"][:, :, c0:c0 + n], h2[:, :, :n], eng="act")
        for s0 in range(0, n, 128):
            m = min(128, n - s0)
            for kc in range(KC):
                k.mm(pr[:m, :], h2[:, kc, s0:s0 + m], rw[:, kc, :], start=(kc == 0), stop=(kc == KC - 1))
            sc = k_router(k, pr, rb, m)
            k.dma(O["cw"][c0 + s0:c0 + s0 + m, :], sc, eng="sp")
    k.emit(); k.close()
    return nc


def k_router(k, pr, rb, m):
    if not hasattr(k, "_rt"):
        k._rt = {n_: k.sb([128, 64]) for n_ in ("sc", "sel", "eq", "sel2", "ge", "cw")}
        k._rt.update({n_: k.sb([128, 8]) for n_ in ("m1", "m2", "gs", "gsel")})
        k._rt.update({n_: k.sb([128, 1]) for n_ in ("gmax", "ws")})
    t = k._rt
    v3 = lambda a: a[:m, :].rearrange("p (g e) -> p g e", e=8)
    b3 = lambda a: a[:m, :].unsqueeze(2).to_broadcast([m, 8, 8])
    k.act(t["sc"][:m, :], pr[:m, :], AF.Sigmoid)
    k.tt("dve", t["sel"][:m, :], t["sc"][:m, :], rb[:m, :], ALU.add)
    k.op("dve", lambda e: e.tensor_reduce(t["m1"][:m, :], v3(t["sel"]), AX.X, ALU.max), R=[t["sel"]], W=[t["m1"]])
    k.tt("dve", v3(t["eq"]), v3(t["sel"]), b3(t["m1"]), ALU.is_equal)
    k.stt(t["sel2"][:m, :], t["eq"][:m, :], -1e9, t["sel"][:m, :], ALU.mult, ALU.add)
    k.op("dve", lambda e: e.tensor_reduce(t["m2"][:m, :], v3(t["sel2"]), AX.X, ALU.max), R=[t["sel2"]], W=[t["m2"]])
    k.tt("dve", t["gs"][:m, :], t["m1"][:m, :], t["m2"][:m, :], ALU.add)
    k.op("dve", lambda e: e.tensor_reduce(t["gmax"][:m, :], t["gs"][:m, :], AX.X, ALU.max), R=[t["gs"]], W=[t["gmax"]])
    k.ts("dve", t["gsel"][:m, :], t["gs"][:m, :], t["gmax"][:m, 0:1], None, ALU.is_equal)
    k.tt("dve", v3(t["ge"]), v3(t["sel"]), b3(t["m2"]), ALU.is_ge)
    k.tt("dve", v3(t["ge"]), v3(t["ge"]), b3(t["gsel"]), ALU.mult)
    k.tt("dve", t["cw"][:m, :], t["ge"][:m, :], t["sc"][:m, :], ALU.mult)
    k.op("dve", lambda e: e.tensor_reduce(t["ws"][:m, :], t["cw"][:m, :], AX.X, ALU.add), R=[t["cw"]], W=[t["ws"]])
    k.recip(t["ws"][:m, :], t["ws"][:m, :])
    k.ts("dve", t["cw"][:m, :], t["cw"][:m, :], t["ws"][:m, 0:1], None, ALU.mult)
    return t["cw"][:m, :]


def build_C(Tx, NE=8, mm_dt=None):
    FR = mybir.dt.float32r
    nc = bass.Bass("TRN2", target_bir_lowering=False)
    k = KB(nc)
    I, O = _mk({"h2T": [128, KC, Tx], "cwT": [NE, Tx], "wg": [NE, 6, 128, KC, 128], "wu": [NE, 6, 128, KC, 128],
                "wd": [NE, 16, 128, 6, 128]}, {"yT": [128, KC, Tx]}, nc)
    N = 512
    h2 = k.sb([128, KC, N]); h2r = k.sb([128, KC, N], FR)
    cwb = k.sb([128, NE, N])
    yacc = k.sb([128, KC, N]); act = k.sb([128, 6, N], FR); sg = [k.sb([128, N]) for _ in range(2)]
    wgb = [k.sb([128, KC, 128]) for _ in range(2)]; wub = [k.sb([128, KC, 128]) for _ in range(2)]
    wgr = k.sb([128, KC, 128], FR); wur = k.sb([128, KC, 128], FR)
    wdb = [k.sb([128, 6, 128]) for _ in range(3)]; wdr = [k.sb([128, 6, 128], FR) for _ in range(2)]
    pg = [k.ps([128, N]) for _ in range(2)]; pu = [k.ps([128, N]) for _ in range(2)]; py = [k.ps([128, N]) for _ in range(3)]
    wi = 0; di = 0
    tiles = [(t0, min(N, Tx - t0)) for t0 in range(0, Tx, N)]
    for ti, (t0, n) in enumerate(tiles):
        k.dma(h2[:, 0:8, :n], I["h2T"][:, 0:8, t0:t0 + n], eng="sp")
        k.dma(h2[:, 8:16, :n], I["h2T"][:, 8:16, t0:t0 + n], eng="act")
        k.dma(cwb[:, :, :n], I["cwT"][:, t0:t0 + n].partition_broadcast(128), eng="pool")
        k.copy("dve", h2r[:, :, :n], h2[:, :, :n])
        for e in range(NE):
            for fc in range(6):
                wg_ = wgb[wi % 2]; wu_ = wub[wi % 2]; p1 = pg[wi % 2]; p2 = pu[wi % 2]; s_ = sg[wi % 2]; wi += 1
                k.dma(wg_[:], I["wg"][e, fc], eng="sp")
                k.dma(wu_[:], I["wu"][e, fc], eng="act")
                k.copy("dve", wgr[:], wg_[:])
                for kc in range(KC):
                    k.mm(p1[:, :n], wgr[:, kc, :], h2r[:, kc, :n], start=(kc == 0), stop=(kc == KC - 1))
                k.copy("dve", wur[:], wu_[:])
                for kc in range(KC):
                    k.mm(p2[:, :n], wur[:, kc, :], h2r[:, kc, :n], start=(kc == 0), stop=(kc == KC - 1))
                k.act(s_[:, :n], p1[:, :n], AF.Silu)
                k.tt("dve", s_[:, :n], s_[:, :n], p2[:, :n], ALU.mult)
                k.tt("dve", act[:, fc, :n], s_[:, :n], cwb[:, e, :n], ALU.mult, wk=fc)
            for dt in range(16):
                wd_ = wdb[di % 3]; wr_ = wdr[di % 2]; p3 = py[di % 3]; di += 1
                k.dma(wd_[:], I["wd"][e, dt], eng="pool")
                k.copy("dve", wr_[:], wd_[:])
                for fc in range(6):
                    k.mm(p3[:, :n], wr_[:, fc, :], act[:, fc, :n], start=(fc == 0), stop=(fc == 5), rk={act.name: fc})
                if e == 0:
                    k.copy("act", yacc[:, dt, :n], p3[:, :n], wk=dt)
                else:
                    k.tt("dve", yacc[:, dt, :n], yacc[:, dt, :n], p3[:, :n], ALU.add, wk=dt, rk={yacc.name: dt})
        k.dma(O["yT"][:, 0:8, t0:t0 + n], yacc[:, 0:8, :n], eng="sp")
        k.dma(O["yT"][:, 8:16, t0:t0 + n], yacc[:, 8:16, :n], eng="act")
    k.emit(); k.close()
    return nc


def build_D(tiles, final):
    NL = sum(t[1] for t in tiles)
    nc = bass.Bass("TRN2", target_bir_lowering=False)
    k = KB(nc)
    I, O = _mk({"yp": [NCORE, 128, KC, NL], "x1T": [128, KC, NL], "vecE": [128, KC, 3]},
               {"x2T": [128, KC, NL]}, nc)
    vec = k.sb([128, KC, 3]); k.dma(vec[:], I["vecE"])
    onesD = k.sb([128, 128]); k.memset("dve", onesD[:], 1.0 / D)
    N = 512
    acc = k.sb([128, KC, N]); x1 = k.sb([128, KC, N]); sq = k.sb([128, KC, N]); rstd = k.sb([128, N])
    yb = [k.sb([128, KC, N]) for _ in range(2)]
    pn = k.ps([128, N])
    for (c0, n, s) in tiles:
        k.dma(x1[:, :, :n], I["x1T"][:, :, c0:c0 + n], eng="pool")
        for j in range(NCORE):
            b = j % 2
            k.dma(yb[b][:, :, :n], I["yp"][j, :, :, c0:c0 + n], eng=("sp" if b == 0 else "act"))
            if j == 0:
                k.copy("dve", acc[:, :, :n], yb[b][:, :, :n])
            else:
                k.tt("dve" if j % 2 else "pool", acc[:, :, :n], acc[:, :, :n], yb[b][:, :, :n], ALU.add)
        k.tt("dve", acc[:, :, :n], acc[:, :, :n], vec[:, :, s:s + 1].to_broadcast([128, KC, n]), ALU.mult)
        k.tt("pool", x1[:, :, :n], x1[:, :, :n], acc[:, :, :n], ALU.add)
        if final:
            k.act(sq[:, :, :n], x1[:, :, :n], AF.Square)
            for kc in range(KC):
                k.mm(pn[:, :n], onesD[:], sq[:, kc, :n], start=(kc == 0), stop=(kc == KC - 1))
            k.ts("dve", rstd[:, :n], pn[:, :n], 1e-6, None, ALU.add)
            k.act(rstd[:, :n], rstd[:, :n], AF.Sqrt)
            k.recip(rstd[:, :n], rstd[:, :n])
            k.tt("dve", x1[:, :, :n], x1[:, :, :n], rstd[:, :n].unsqueeze(1).to_broadcast([128, KC, n]), ALU.mult)
            k.tt("pool", x1[:, :, :n], x1[:, :, :n], vec[:, :, 2:3].to_broadcast([128, KC, n]), ALU.mult)
        k.dma(O["x2T"][:, :, c0:c0 + n], x1[:, :, :n], eng="sp")
    k.emit(); k.close()
    return nc


def rows_fm(a):
    R, Tn = a.shape
    return a.reshape(R // 128, 128, Tn).transpose(1, 0, 2)


def _run(nc, maps):
    return run_bass_kernel_spmd(nc, maps, core_ids=list(range(len(maps)))).results


def _tiles(nctx, nlat, n):
    out = [(0, nctx, 1)]
    t = 0
    while t < nlat:
        m_ = min(n, nlat - t)
        out.append((nctx + t, m_, 0)); t += m_
    return out


def forward(inp, dbg=None):
    inp = {k_: np.asarray(v) for k_, v in inp.items()}
    LCx = inp["ctx"].shape[1]; LLx = inp["x"].shape[1]; Tx = LCx + LLx
    nctx = LCx // NCORE; nlat = LLx // NCORE; NL = nctx + nlat
    m, x0T = run_M(inp)
    xfull = np.ascontiguousarray(np.concatenate([fm(inp["ctx"][0]), x0T], axis=2))
    idx = [np.concatenate([np.arange(nctx * j, nctx * (j + 1)), LCx + np.arange(nlat * j, nlat * (j + 1))])
           for j in range(NCORE)]
    tilesB = _tiles(nctx, nlat, 256); tilesD = _tiles(nctx, nlat, 512)
    ncB = build_B(tilesB); ncC = build_C(Tx)
    rw = np.ascontiguousarray(inp["router_w"].reshape(KC, 128, 64).transpose(1, 0, 2))
    rb = np.ascontiguousarray(np.broadcast_to(inp["router_b"][None, :], (128, 64)))
    for l in range(2):
        last = (l == 1)
        mm_ = m[l].reshape(2, 6, D)
        ncA = build_A(l, LCx, LLx)
        rA = _run(ncA, [a_host_inputs(inp, l, j, xfull, m) for j in range(NCORE)])
        brT = np.concatenate([np.concatenate([rA[j][nm] for j in range(NCORE)], 0) for nm in ("yA", "yB", "yC", "yD")], 0)
        ogT = np.concatenate([rA[j]["ogT"] for j in range(NCORE)], 0)
        gT = np.concatenate([rA[j]["gT"] for j in range(NCORE)], 0)
        if dbg is not None:
            dbg[f"brT{l}"] = brT; dbg[f"gT{l}"] = gT
        del rA
        brF = rows_fm(brT); ogF = rows_fm(ogT); gF = rows_fm(gT)
        wbr = np.ascontiguousarray(inp["w_branch"][l].reshape(16, 128, 16, 128).transpose(2, 1, 0, 3))
        wout = np.ascontiguousarray(inp["w_out"][l].reshape(16, 128, 16, 128).transpose(2, 1, 0, 3))
        vecB = np.ascontiguousarray(np.stack([fm(mm_[0, 2]), fm(mm_[1, 2]), fm(inp["norm_ffn_g"][l]), fm(mm_[0, 3]),
                                              fm(mm_[0, 4]), fm(mm_[1, 3]), fm(mm_[1, 4]), fm(mm_[1, 4])], axis=-1))
        hgn = np.ascontiguousarray(inp["hg_norm_g"][l][:, None])
        mapsB = [{"xT": np.ascontiguousarray(xfull[:, :, idx[j]]), "brT": np.ascontiguousarray(brF[:, :, idx[j]]),
                  "ogT": np.ascontiguousarray(ogF[:, :, idx[j]]), "gT": np.ascontiguousarray(gF[:, :, idx[j]]),
                  "wbr": wbr, "wout": wout, "vecB": vecB, "hgn": hgn, "rw": rw, "rb": rb} for j in range(NCORE)]
        del brF, ogF, gF, gT, brT, ogT
        rB = _run(ncB, mapsB)
        del mapsB
        h2full = np.empty((128, KC, Tx), np.float32); cwfull = np.empty((Tx, 64), np.float32)
        for j in range(NCORE):
            h2full[:, :, idx[j]] = rB[j]["h2T"]; cwfull[idx[j], :] = rB[j]["cw"]
        if dbg is not None:
            dbg[f"h2_{l}"] = h2full.copy(); dbg[f"cw_{l}"] = cwfull.copy()
            dbg[f"x1_{l}"] = np.concatenate([rB[j]["x1T"] for j in range(NCORE)], 2)
        mapsC = []
        for j in range(NCORE):
            es = slice(8 * j, 8 * j + 8)
            wg = inp["moe_w_gate"][l, es].reshape(8, KC, 128, 6, 128).transpose(0, 3, 2, 1, 4)
            wu = inp["moe_w_up"][l, es].reshape(8, KC, 128, 6, 128).transpose(0, 3, 2, 1, 4)
            wd = inp["moe_w_down"][l, es].reshape(8, 6, 128, 16, 128).transpose(0, 3, 2, 1, 4)
            mapsC.append({"h2T": h2full, "cwT": np.ascontiguousarray(cwfull[:, es].T), "wg": np.ascontiguousarray(wg),
                          "wu": np.ascontiguousarray(wu), "wd": np.ascontiguousarray(wd)})
        rC = _run(ncC, mapsC)
        del mapsC
        vecE = np.ascontiguousarray(np.stack([fm(mm_[0, 5]), fm(mm_[1, 5]), fm(inp["final_norm_g"])], axis=-1))
        mapsD = [{"yp": np.ascontiguousarray(np.stack([rC[jj]["yT"][:, :, idx[j]] for jj in range(NCORE)], 0)),
                  "x1T": rB[j]["x1T"], "vecE": vecE} for j in range(NCORE)]
        del rC
        ncD = build_D(tilesD, last)
        rD = _run(ncD, mapsD)
        del mapsD
        for j in range(NCORE):
            xfull[:, :, idx[j]] = rD[j]["x2T"]
        if dbg is not None:
            dbg[f"x2_{l}"] = xfull.copy()
    out = xfull[:, :, LCx:].transpose(2, 1, 0).reshape(LLx, D)[None]
    return np.ascontiguousarray(out.astype(np.float32))


def kernel(**inputs):
    return forward(inputs)
```
